# Optimizing a Trainium2 kernel written in Bass

```python
import jax, jax.numpy as jnp
from jax import lax
import numpy as np

D_MODEL = 1024
BATCH = 8
SEQ = 2048
DEPTH = 2

CHUNK = 64
D_MIX = D_MODEL
POOL_WINDOWS = (2, 4, 8, 16)
N_POOL_GROUPS = len(POOL_WINDOWS)
D_POOL = D_MIX // 4
POOL_GC = D_POOL // N_POOL_GROUPS
D_SGU = D_MIX // 2
SGU_HEADS = 4
SGU_HD = D_SGU // SGU_HEADS
SGU_BLOCK = 128
D_CONV = D_MIX - D_POOL - D_SGU
CONV_W = 3
D_IN = D_POOL + 2 * D_SGU + 3 * D_CONV
SPLITS = (D_POOL, D_POOL + D_SGU, D_POOL + 2 * D_SGU, D_POOL + 2 * D_SGU + D_CONV, D_POOL + 2 * D_SGU + 2 * D_CONV)
D_FF = (7 * D_MODEL) // 2
N_EXPERTS = 8
TOP_K = 2
N_DENSE = (DEPTH + 1) // 2
N_MOE = DEPTH // 2
EPS = 1e-6

kernel_name = "hybrid_pool_sgu_shortconv_moe"


def rmsnorm(x, g):
    xf = x.astype(jnp.float32)
    y = xf * lax.rsqrt(jnp.mean(xf * xf, axis=-1, keepdims=True) + EPS)
    return (y * g.astype(jnp.float32)).astype(x.dtype)


def layernorm(x, g, b):
    xf = x.astype(jnp.float32)
    mu = jnp.mean(xf, axis=-1, keepdims=True)
    var = jnp.mean(jnp.square(xf - mu), axis=-1, keepdims=True)
    y = (xf - mu) * lax.rsqrt(var + EPS)
    return (y * g.astype(jnp.float32) + b.astype(jnp.float32)).astype(x.dtype)


def swiglu(h, w_gate, w_up, w_down):
    return (jax.nn.silu(h @ w_gate) * (h @ w_up)) @ w_down


def pool_mixer(a, pool_w, pool_scale):
    bsz, seq, _ = a.shape
    af = a.astype(jnp.float32).reshape(bsz, seq, N_POOL_GROUPS, POOL_GC)
    cs = jnp.cumsum(af, axis=1)
    pos = jnp.arange(seq)
    outs = []
    for g, w in enumerate(POOL_WINDOWS):
        c = cs[:, :, g]
        c_prev = jnp.pad(c, ((0, 0), (w, 0), (0, 0)))[:, :seq]
        count = jnp.minimum(pos + 1, w).astype(jnp.float32)[None, :, None]
        outs.append((c - c_prev) / count - af[:, :, g])
    d = jnp.stack(outs, axis=2).astype(a.dtype)
    y = jnp.einsum("bsgc,gcd->bsgd", d, pool_w)
    return y.reshape(bsz, seq, D_POOL) * pool_scale


def sgu_mixer(u, v, ln_g, ln_b, w_s, b_s):
    bsz, seq, _ = u.shape
    n_blk = seq // SGU_BLOCK
    vn = layernorm(v, ln_g, ln_b).reshape(bsz, n_blk, SGU_BLOCK, SGU_HEADS, SGU_HD)
    cid = jnp.arange(SGU_BLOCK) // CHUNK
    mask = (cid[:, None] >= cid[None, :]).astype(w_s.dtype)
    mixed = jnp.einsum("hij,bnjhc->bnihc", w_s * mask, vn) + b_s.T[None, None, :, :, None]
    return u * mixed.reshape(bsz, seq, D_SGU)


def short_conv_mixer(gb, gc, xc, conv_w):
    z = gc * xc
    zc = lax.conv_general_dilated(
        z, conv_w[:, None, :], window_strides=(1,), padding=[(CONV_W - 1, 0)],
        dimension_numbers=("NWC", "WIO", "NWC"), feature_group_count=D_CONV)
    return gb * zc


def token_mixer(h, w_in, pool_w, pool_scale, sgu_ln_g, sgu_ln_b, sgu_w, sgu_b, conv_w, group_g, w_out):
    p = h @ w_in
    a, u, v, gb, gc, xc = jnp.split(p, list(SPLITS), axis=-1)
    y_a = pool_mixer(a, pool_w, pool_scale)
    y_b = sgu_mixer(u, v, sgu_ln_g, sgu_ln_b, sgu_w, sgu_b)
    y_c = short_conv_mixer(gb, gc, xc, conv_w)
    y = jnp.concatenate([
        rmsnorm(y_a, group_g[:D_POOL]),
        rmsnorm(y_b, group_g[D_POOL:D_POOL + D_SGU]),
        rmsnorm(y_c, group_g[D_POOL + D_SGU:]),
    ], axis=-1)
    return y @ w_out


def moe_ffn(h, router_w, router_b, w_gate, w_up, w_down):
    bsz, seq, d = h.shape
    t = h.reshape(bsz * seq, d)
    logits = t.astype(jnp.float32) @ router_w.astype(jnp.float32) + router_b.astype(jnp.float32)
    top_v, top_i = lax.top_k(logits, TOP_K)
    gates = jax.nn.softmax(top_v, axis=-1)
    combine = jnp.sum(jax.nn.one_hot(top_i, N_EXPERTS, dtype=jnp.float32) * gates[..., None], axis=1)
    out = jnp.zeros((bsz * seq, d), jnp.float32)
    for e in range(N_EXPERTS):
        y_e = swiglu(t, w_gate[e], w_up[e], w_down[e])
        out = out + combine[:, e:e + 1] * y_e.astype(jnp.float32)
    return out.astype(h.dtype).reshape(bsz, seq, d)


def setup_inputs(seed: int = 0) -> dict:
    key = jax.random.key(seed)
    ks = jax.random.split(key, 24)

    def nrm(k, shape, scale):
        return jax.random.normal(k, shape, jnp.float32) * scale

    return {
        "x": nrm(ks[0], (BATCH, SEQ, D_MODEL), 1.0),
        "norm1_g": 1.0 + nrm(ks[1], (DEPTH, D_MODEL), 0.05),
        "w_in": nrm(ks[2], (DEPTH, D_MODEL, D_IN), D_MODEL ** -0.5),
        "pool_w": nrm(ks[3], (DEPTH, N_POOL_GROUPS, POOL_GC, POOL_GC), POOL_GC ** -0.5),
        "pool_scale": 1.0 + nrm(ks[4], (DEPTH, D_POOL), 0.1),
        "sgu_ln_g": 1.0 + nrm(ks[5], (DEPTH, D_SGU), 0.05),
        "sgu_ln_b": nrm(ks[6], (DEPTH, D_SGU), 0.02),
        "sgu_w": nrm(ks[7], (DEPTH, SGU_HEADS, SGU_BLOCK, SGU_BLOCK), SGU_BLOCK ** -0.5),
        "sgu_b": 1.0 + nrm(ks[8], (DEPTH, SGU_HEADS, SGU_BLOCK), 0.05),
        "conv_w": nrm(ks[9], (DEPTH, CONV_W, D_CONV), CONV_W ** -0.5),
        "group_g": 1.0 + nrm(ks[10], (DEPTH, D_MIX), 0.05),
        "w_out": nrm(ks[11], (DEPTH, D_MIX, D_MODEL), D_MIX ** -0.5),
        "norm2_g": 1.0 + nrm(ks[12], (DEPTH, D_MODEL), 0.05),
        "ffn_w_gate": nrm(ks[13], (N_DENSE, D_MODEL, D_FF), D_MODEL ** -0.5),
        "ffn_w_up": nrm(ks[14], (N_DENSE, D_MODEL, D_FF), D_MODEL ** -0.5),
        "ffn_w_down": nrm(ks[15], (N_DENSE, D_FF, D_MODEL), D_FF ** -0.5),
        "router_w": nrm(ks[16], (N_MOE, D_MODEL, N_EXPERTS), D_MODEL ** -0.5),
        "router_b": nrm(ks[17], (N_MOE, N_EXPERTS), 0.01),
        "moe_w_gate": nrm(ks[18], (N_MOE, N_EXPERTS, D_MODEL, D_FF), D_MODEL ** -0.5),
        "moe_w_up": nrm(ks[19], (N_MOE, N_EXPERTS, D_MODEL, D_FF), D_MODEL ** -0.5),
        "moe_w_down": nrm(ks[20], (N_MOE, N_EXPERTS, D_FF, D_MODEL), D_FF ** -0.5),
        "final_g": 1.0 + nrm(ks[21], (D_MODEL,), 0.05),
    }


def reference(x, norm1_g, w_in, pool_w, pool_scale, sgu_ln_g, sgu_ln_b, sgu_w, sgu_b, conv_w,
              group_g, w_out, norm2_g, ffn_w_gate, ffn_w_up, ffn_w_down, router_w, router_b,
              moe_w_gate, moe_w_up, moe_w_down, final_g):
    for l in range(DEPTH):
        h = rmsnorm(x, norm1_g[l])
        x = x + token_mixer(h, w_in[l], pool_w[l], pool_scale[l], sgu_ln_g[l], sgu_ln_b[l],
                            sgu_w[l], sgu_b[l], conv_w[l], group_g[l], w_out[l])
        h = rmsnorm(x, norm2_g[l])
        j = l // 2
        if l % 2 == 0:
            x = x + swiglu(h, ffn_w_gate[j], ffn_w_up[j], ffn_w_down[j])
        else:
            x = x + moe_ffn(h, router_w[j], router_b[j], moe_w_gate[j], moe_w_up[j], moe_w_down[j])
    return rmsnorm(x, final_g)
```

```python
import bisect
import numpy as np
import concourse.bass as bass
import concourse.mybir as mybir
from concourse.bass_utils import run_bass_kernel_spmd

F32 = mybir.dt.float32
BF16 = mybir.dt.bfloat16
ALU = mybir.AluOpType
AF = mybir.ActivationFunctionType
AX = mybir.AxisListType

D = 1024
T = 2048
KC = 8
TT = 512
NTT = 4
DIN = 2048
DFF = 3584
FGC = 4
FGW = FGC * 128
NFG = DFF // FGW
NE = 8
EPS = 1e-6
PL = 32
NPAR = 2 * PL + 8 + 8 + 2 + 32
C_FG = 2 * PL
C_RB = C_FG + 8
C_INVW = C_RB + 8
C_INVC = C_INVW + 2
SB_BASE = 16512
SB_END = 229376


class _Op:
    __slots__ = ("eng", "fn", "deps", "dma", "signal", "sigval", "idx", "seg")


class Prog:
    def __init__(self, nc):
        self.nc = nc
        self.ops = []
        self.last_writer = {}
        self.readers = {}
        self.last_on_eng = {}
        self.pending_bar = {}
        self.dangling_dma = set()
        self.seg = 0
        self.seg_end = {}

    def segment(self, seg):
        self.seg_end[self.seg] = set(self.last_on_eng.values()) | set(self.dangling_dma)
        self.seg = seg
        self.last_writer = {}
        self.readers = {}
        self.last_on_eng = {}
        self.pending_bar = {}
        self.dangling_dma = set()

    def op(self, eng, fn, reads=(), writes=(), dma=None):
        o = _Op()
        o.eng, o.fn, o.dma = eng, fn, dma
        o.seg = self.seg
        o.signal = dma is not None
        o.sigval = None
        o.idx = len(self.ops)
        writes = list(writes) + [r for r in reads if isinstance(r, tuple) and r[0] == "ps" and r not in writes]
        deps = set()
        for r in reads:
            w = self.last_writer.get(r)
            if w is not None:
                deps.add(w)
        for w_ in writes:
            w = self.last_writer.get(w_)
            if w is not None:
                deps.add(w)
            for rd in self.readers.get(w_, ()):
                deps.add(rd)
        pb = self.pending_bar.pop(eng, None)
        if pb:
            deps |= pb
        deps.discard(o.idx)
        o.deps = deps
        for d in deps:
            self.dangling_dma.discard(d)
        for r in reads:
            self.readers.setdefault(r, []).append(o.idx)
        for w_ in writes:
            self.last_writer[w_] = o.idx
            self.readers[w_] = []
        if dma is not None:
            self.dangling_dma.add(o.idx)
        else:
            self.last_on_eng[eng] = o.idx
        self.ops.append(o)
        return o.idx

    def barrier(self):
        deps = set(self.last_on_eng.values()) | set(self.dangling_dma)
        for e in ("pe", "act", "dve", "pool", "sp"):
            self.pending_bar[e] = set(deps) | self.pending_bar.get(e, set())
        self.dangling_dma = set()

    def emit(self, final_waits=(), flag_ap=None, dummies=None):
        nc = self.nc
        ops = self.ops
        for idx in final_waits:
            ops[idx].signal = True
        for sg_, deps_ in self.seg_end.items():
            for d in deps_:
                ops[d].signal = True
        for o in ops:
            nd = set()
            for d in o.deps:
                p = ops[d]
                assert p.seg == o.seg
                if p.dma is None and o.dma is None and p.eng == "pe" and o.eng == "pe":
                    continue
                nd.add(d)
            o.deps = nd
            for d in nd:
                ops[d].signal = True
        counters = {}
        semkeys = []
        dma_hist = {}
        for o in ops:
            if not o.signal:
                continue
            key = (o.seg, "dma", o.dma) if o.dma is not None else (o.seg, "eng", o.eng)
            if key not in counters:
                counters[key] = 0
                semkeys.append(key)
            counters[key] += 16 if o.dma is not None else 1
            o.sigval = (key, counters[key])
            if o.dma is not None:
                dma_hist.setdefault(key, ([], []))
                dma_hist[key][0].append(o.idx)
                dma_hist[key][1].append(counters[key])
        sems = {}
        for key in semkeys:
            sems[key] = nc.alloc_semaphore("s%d_%s_%s" % key)
        engobj = ("sp", "pool", "act", "dve", "pe")
        per_eng = {e: {0: [], 1: [], 2: [], 3: []} for e in engobj}
        for o in ops:
            per_eng[o.eng][o.seg].append(o)
        branching = any(o.seg in (1, 2) for o in ops)
        bar0 = nc.alloc_semaphore("bar0") if branching else None
        bar1 = nc.alloc_semaphore("bar1") if branching else None
        nwaits = [0]

        def waitval(consumer_idx, d):
            key, val = ops[d].sigval
            if key[1] == "dma":
                idxs, vals = dma_hist[key]
                pos = bisect.bisect_left(idxs, consumer_idx) - 1
                val = max(val, vals[pos])
            return key, val

        def emit_ops(eng, lst):
            waited = {}
            for o in lst:
                need = {}
                for d in o.deps:
                    key, val = waitval(o.idx, d)
                    if need.get(key, 0) < val:
                        need[key] = val
                for key, val in need.items():
                    if waited.get(key, 0) >= val:
                        continue
                    eng.wait_ge(sems[key], val)
                    waited[key] = val
                    nwaits[0] += 1
                ins = o.fn(eng)
                if o.signal:
                    ins.then_inc(sems[o.sigval[0]], 16 if o.dma is not None else 1)

        def wait_seg_end(eng, seg):
            need = {}
            for d in self.seg_end.get(seg, ()):
                key, val = ops[d].sigval
                if key[1] == "dma":
                    val = max(val, dma_hist[key][1][-1] if False else val)
                if need.get(key, 0) < val:
                    need[key] = val
            for key, val in need.items():
                eng.wait_ge(sems[key], val)
                nwaits[0] += 1

        def end_seg(eng, ename, bsem):
            if ename == "act":
                eng.copy(out=dummies["act"][:, 0:1], in_=dummies["act"][:, 1:2]).then_inc(bsem, 1)
            elif ename == "dve":
                eng.tensor_copy(out=dummies["dve"][:, 0:1], in_=dummies["dve"][:, 1:2]).then_inc(bsem, 1)
            elif ename == "pool":
                eng.memset(dummies["pool"][:, 0:1], 0.0).then_inc(bsem, 1)

        with nc.Block() as block:
            deco = {"pe": block.tensor, "act": block.scalar, "dve": block.vector,
                    "pool": block.gpsimd, "sp": block.sync}
            for ename in engobj:
                segs = per_eng[ename]

                def body(eng, segs=segs, ename=ename):
                    emit_ops(eng, segs[0])
                    if branching:
                        end_seg(eng, ename, bar0)
                        eng.wait_ge(bar0, 3)
                        wait_seg_end(eng, 0)
                        reg = eng.alloc_register("flag_" + ename)
                        eng.reg_load(reg, flag_ap)
                        with eng.If_eq(reg, 0):
                            emit_ops(eng, segs[1])
                            wait_seg_end(eng, 1)
                            end_seg(eng, ename, bar1)
                        with eng.Else():
                            emit_ops(eng, segs[2])
                            wait_seg_end(eng, 2)
                            end_seg(eng, ename, bar1)
                        eng.wait_ge(bar1, 3)
                    emit_ops(eng, segs[3])
                    if ename == "sp":
                        for idx in final_waits:
                            key, val = ops[idx].sigval
                            eng.wait_ge(sems[key], val)

                deco[ename](body)
        return nwaits[0]


class Region:
    def __init__(self, nc, base, size):
        self.nc, self.base, self.size = nc, base, size
        self.off = 0
        self.n = 0

    def reset(self):
        self.off = 0

    def alloc(self, name, shape, dtype):
        esz = 2 if dtype == BF16 else 4
        nbytes = esz
        for s in shape[1:]:
            nbytes *= s
        nbytes = (nbytes + 31) // 32 * 32
        assert self.off + nbytes <= self.size, (name, self.off, nbytes, self.size)
        self.n += 1
        t = self.nc.alloc_sbuf_tensor_at("%s_%d" % (name, self.n), list(shape), dtype,
                                         offset=self.base + self.off)
        self.off += nbytes
        return t


def build_nc(stage=99, PARTS=("sgu", "conv", "pool", "wout"), NE_RUN=NE, ROUTED=True, FORCE_DENSE=False, DEBUG=False):
    nc = bass.Bass("TRN2", target_bir_lowering=False)
    dt = lambda n, s: nc.dram_tensor(n, s, F32, kind="ExternalInput").ap()
    xT_d = dt("xT", [D, T])
    w_in_d = dt("w_in", [2, D, DIN])
    w_out_d = dt("w_out", [2, D, D])
    if stage >= 2:
        fwg_d = dt("ffn_wg", [D, DFF])
        fwu_d = dt("ffn_wu", [D, DFF])
        fwd_d = dt("ffn_wd", [DFF, D])
    if stage >= 4:
        mwg_d = dt("moe_wg", [NE_RUN, D, DFF])
        mwu_d = dt("moe_wu", [NE_RUN, D, DFF])
        mwd_d = dt("moe_wd", [NE_RUN, DFF, D])
    rw_d = dt("router_wl", [128, KC, NE])
    par_d = dt("params", [128, NPAR])
    bc_d = dt("bc", [2, 3, 128, 512])
    swT_d = dt("sgu_wT", [2, 128, 4, 128])
    pw_d = dt("pool_w", [2, 4, 64, 64])
    outT_d = nc.dram_tensor("outT", [D, T], F32, kind="ExternalOutput").ap()
    dbg_d = nc.dram_tensor("dbg", [128, 2], F32, kind="ExternalOutput").ap() if DEBUG else None

    pers = Region(nc, SB_BASE, SB_END - SB_BASE)
    xT = pers.alloc("xT", [128, KC, T], F32)
    hcT_off = SB_BASE + pers.off
    hcT = pers.alloc("hcT", [128, KC, T], BF16)
    hTok = nc.alloc_sbuf_tensor_at("hTok", [128, 16, D], BF16, offset=hcT_off)
    par = pers.alloc("par", [128, NPAR], F32)
    ident = pers.alloc("ident", [128, 128], F32)
    ones_bf = pers.alloc("ones_bf", [128, 128], BF16)
    ones_f = pers.alloc("ones_f", [128, 128], F32)
    rw = pers.alloc("rw", [128, KC, NE], F32)
    comb = pers.alloc("comb", [128, 16, NE], F32)
    posm = pers.alloc("posm", [128, 16, NE], F32)
    ghl = pers.alloc("ghl", [128, 16, NE, 2], BF16)
    ltri_bf = pers.alloc("ltri_bf", [128, 128], BF16)
    flagf = pers.alloc("flagf", [128, 1], F32)
    flagi = pers.alloc("flagi", [128, 1], mybir.dt.int32)
    dummies = {"act": pers.alloc("dumA", [128, 2], F32), "dve": pers.alloc("dumD", [128, 2], F32),
               "pool": pers.alloc("dumP", [128, 2], F32)}
    dbg = pers.alloc("dbg", [128, 2], F32)
    hT_off = SB_BASE + pers.off
    hT = pers.alloc("hT", [128, KC, T], BF16)
    R = Region(nc, SB_BASE + pers.off, SB_END - SB_BASE - pers.off)
    psum = [nc.alloc_psum_tensor("ps%d" % i, [128, 512], F32) for i in range(8)]

    P = Prog(nc)
    psc = [0]

    def nextps():
        b = psc[0] % 8
        psc[0] += 1
        return b

    def tsl(j):
        return slice(j * TT, (j + 1) * TT)

    def mm(out, lhsT, rhs, start, stop, reads, writes):
        P.op("pe", lambda e: e.matmul(out, lhsT=lhsT, rhs=rhs, start=start, stop=stop), reads, writes)

    def dma(q, out, in_, key, reads=(), writes=()):
        return P.op(q, lambda e: e.dma_start(out=out, in_=in_), reads, writes, dma=key)

    def tt(eng, out, in0, in1, op, reads, writes):
        P.op(eng, lambda e: e.tensor_tensor(out=out, in0=in0, in1=in1, op=op), reads, writes)

    def stt(eng, out, in0, scalar, in1, op0, op1, reads, writes):
        P.op(eng, lambda e: e.scalar_tensor_tensor(out=out, in0=in0, scalar=scalar, in1=in1, op0=op0, op1=op1),
             reads, writes)

    def ts(eng, out, in0, s1, s2, op0, op1, reads, writes):
        if s2 is None:
            P.op(eng, lambda e: e.tensor_scalar(out=out, in0=in0, scalar1=s1, scalar2=None, op0=op0), reads, writes)
        else:
            P.op(eng, lambda e: e.tensor_scalar(out=out, in0=in0, scalar1=s1, scalar2=s2, op0=op0, op1=op1),
                 reads, writes)

    def act(out, in_, func, reads, writes, bias=None, scale=None, accum_out=None):
        kw = {}
        if bias is not None:
            kw["bias"] = bias
        if scale is not None:
            kw["scale"] = scale
        if accum_out is not None:
            kw["accum_out"] = accum_out
        P.op("act", lambda e: e.activation(out=out, in_=in_, func=func, **kw), reads, writes)

    def recip(out, in_, reads, writes):
        P.op("dve", lambda e: e.reciprocal(out=out, in_=in_), reads, writes)

    def memset(eng, ap, val, writes):
        P.op(eng, lambda e: e.memset(ap, val), (), writes)

    dma("sp", xT[:], xT_d.rearrange("(k p) t -> p k t", p=128), "xT", writes=[("x", k, j) for k in range(KC) for j in range(NTT)])
    dma("sp", par[:], par_d, "par", writes=["par"])
    dma("sp", rw[:], rw_d, "rw", writes=["rw"])
    memset("pool", ident[:], 0.0, ["ident"])
    P.op("pool", lambda e: e.affine_select(out=ident[:], in_=ident[:], compare_op=ALU.not_equal, fill=1.0,
                                           base=0, pattern=[[-1, 128]], channel_multiplier=1),
         ["ident"], ["ident"])
    memset("pool", ones_bf[:], 1.0, ["ones_bf"])
    memset("pool", ones_f[:], 1.0, ["ones_f"])
    for dn in ("act", "dve", "pool"):
        memset("pool", dummies[dn][:], 0.0, ["dum" + dn])

    def group_rstd(srcs, src_tokens, nchan, sq, rs, tag):
        n = len(srcs)
        for i, (ap, tok) in enumerate(srcs):
            act(sq[:, i, :], ap, AF.Square, [tok], [(tag + "sq", i)])
        b = nextps()
        for i in range(n):
            mm(psum[b][:], ones_bf[:], sq[:, i, :], i == 0, i == n - 1, [(tag + "sq", i), "ones_bf"], [("ps", b)])
        act(rs[:], psum[b][:], AF.Sqrt, [("ps", b)], [tag + "rs"], bias=EPS, scale=1.0 / nchan)
        recip(rs[:], rs[:], [tag + "rs"], [tag + "rs"])

    def rmsnorm_full(gcol, dst, dst_tag, hook=None):
        P.barrier()
        R.reset()
        sqs = [R.alloc("nsq", [128, KC, TT], BF16) for _ in range(2)]
        rss = [R.alloc("nrs", [128, TT], F32) for _ in range(2)]
        extra = hook("alloc") if hook else None
        for j in range(NTT):
            sq, rs = sqs[j % 2], rss[j % 2]
            tag = "n%d" % (j % 2)
            group_rstd([(xT[:, k, tsl(j)], ("x", k, j)) for k in range(KC)], None, D, sq, rs, tag)
            if hook:
                hook("tile", j, rs, tag, extra)
            if dst is not None:
                for k in range(KC):
                    stt("dve", dst[:, k, tsl(j)], xT[:, k, tsl(j)], par[:, gcol + k:gcol + k + 1], rs[:],
                        ALU.mult, ALU.mult, [("x", k, j), "par", tag + "rs"], [(dst_tag, k, j)])

    def mixer(l):
        pb = l * PL
        C_N1, C_PSC, C_CW, C_GG = pb, pb + 8, pb + 10, pb + 16
        w_in_l = w_in_d[l].rearrange("(k p) n -> p k n", p=128)

        def load_w(name, c0, c1):
            wt = R.alloc(name, [128, KC, c1 - c0], BF16)
            dma("pool", wt[:], w_in_l[:, :, c0:c1], name, writes=[name])
            return wt

        def proj_fm(wt, name, col0, j, b):
            for k in range(KC):
                mm(psum[b][:], wt[:, k, col0:col0 + 128], hT[:, k, tsl(j)], k == 0, k == KC - 1,
                   [name, ("h", k, j)], [("ps", b)])

        def gnorm(ybuf, ytag, nch, gbase, ycat_c0, sqs, rss, j):
            sq, rs = sqs[j % 2], rss[j % 2]
            tag = ytag + "g%d" % (j % 2)
            group_rstd([(ybuf[:, c, tsl(j)], (ytag, c, j)) for c in range(nch)], None, nch * 128, sq, rs, tag)
            for c in range(nch):
                kk = ycat_c0 + c
                stt("dve", hcT[:, kk, tsl(j)], ybuf[:, c, tsl(j)], par[:, C_GG + kk:C_GG + kk + 1], rs[:],
                    ALU.mult, ALU.mult, [(ytag, c, j), "par", tag + "rs"], [("yc", kk, j)])

        for hf in (range(2) if "sgu" in PARTS else ()):
            P.barrier()
            R.reset()
            wv = load_w("wv", 768, 1280)
            wu = load_w("wu", 256, 768)
            lng = R.alloc("lng", [128, 512], F32)
            lnb = R.alloc("lnb", [128, 512], F32)
            sbb = R.alloc("sbb", [128, 512], F32)
            dma("sp", lng[:], bc_d[l, 0], "lng", writes=["lng"])
            dma("sp", lnb[:], bc_d[l, 1], "lnb", writes=["lnb"])
            dma("sp", sbb[:], bc_d[l, 2], "sbb", writes=["sbb"])
            wmT = R.alloc("wmT", [128, 4, 128], BF16)
            dma("pool", wmT[:], swT_d[l], "wmT", writes=["wmT"])
            memset("dve", wmT[64:128, :, 0:64], 0.0, ["wmT"])
            zn = R.alloc("zn", [128, 8, 512], BF16)
            mixb = R.alloc("mixb", [128, 4, 1024], F32)
            vt = [R.alloc("vt", [128, 512], F32) for _ in range(2)]
            junk = R.alloc("junk", [128, 512], BF16)
            st = R.alloc("st", [128, 8, 8], F32)
            memset("dve", st[:], 0.0, ["st"])
            sqs = [R.alloc("bsq", [128, 4, TT], BF16) for _ in range(2)]
            rss = [R.alloc("brs", [128, TT], F32) for _ in range(2)]
            for n in range(8):
                tok0 = hf * 1024 + n * 128
                jt = tok0 // TT
                b = nextps()
                for k in range(KC):
                    mm(psum[b][:], hT[:, k, tok0:tok0 + 128], wv[:, k, :], k == 0, k == KC - 1,
                       [("h", k, jt), "wv"], [("ps", b)])
                s_sum, s_ssq = st[:, n, 0:1], st[:, n, 1:2]
                s_mean, s_m2, s_var, s_rstd = st[:, n, 2:3], st[:, n, 3:4], st[:, n, 4:5], st[:, n, 5:6]
                stn = ("st", n)
                P.op("dve", lambda e, o=s_sum, i=psum[b][:]: e.tensor_reduce(out=o, in_=i, axis=AX.X, op=ALU.add),
                     [("ps", b), "st"], [(stn, 0)])
                act(junk[:], psum[b][:], AF.Square, [("ps", b), "st"], [(stn, 1), "junk"], accum_out=s_ssq)
                ts("dve", s_mean, s_sum, 1.0 / 512, None, ALU.mult, None, [(stn, 0)], [(stn, 2)])
                tt("dve", s_m2, s_mean, s_mean, ALU.mult, [(stn, 2)], [(stn, 3)])
                stt("dve", s_var, s_ssq, 1.0 / 512, s_m2, ALU.mult, ALU.subtract, [(stn, 1), (stn, 3)], [(stn, 4)])
                act(s_rstd, s_var, AF.Sqrt, [(stn, 4)], [(stn, 5)], bias=EPS)
                recip(s_rstd, s_rstd, [(stn, 5)], [(stn, 5)])
                v = vt[n % 2]
                vtag = ("vt", n % 2)
                ts("dve", v[:], psum[b][:], s_mean, s_rstd, ALU.subtract, ALU.mult,
                   [("ps", b), (stn, 2), (stn, 5)], [vtag])
                tt("pool", v[:], v[:], lng[:], ALU.mult, [vtag, "lng"], [vtag])
                tt("pool", zn[:, n, :], v[:], lnb[:], ALU.add, [vtag, "lnb"], [("zn", n)])
                b2 = nextps()
                for h in range(4):
                    mm(psum[b2][:, h * 128:(h + 1) * 128], zn[:, n, h * 128:(h + 1) * 128], wmT[:, h, :], True, True,
                       [("zn", n), "wmT"], [("ps", b2)])
                tt("dve", mixb[:, :, n * 128:(n + 1) * 128], psum[b2][:].rearrange("p (h i) -> p h i", h=4),
                   sbb[:].rearrange("p (h i) -> p h i", h=4), ALU.add, [("ps", b2), "sbb"], [("mixb", n)])
            for jj in range(2):
                j = hf * 2 + jj
                for h in range(4):
                    b = nextps()
                    proj_fm(wu, "wu", h * 128, j, b)
                    tt("dve", mixb[:, h, jj * TT:(jj + 1) * TT], psum[b][:], mixb[:, h, jj * TT:(jj + 1) * TT], ALU.mult,
                       [("ps", b)] + [("mixb", jj * 4 + q) for q in range(4)], [("yb", h, jj)])
                sq, rs = sqs[jj], rss[jj]
                tag = "gb%d" % jj
                group_rstd([(mixb[:, h, jj * TT:(jj + 1) * TT], ("yb", h, jj)) for h in range(4)], None, 512, sq, rs, tag)
                for h in range(4):
                    kk = 2 + h
                    stt("dve", hcT[:, kk, tsl(j)], mixb[:, h, jj * TT:(jj + 1) * TT], par[:, C_GG + kk:C_GG + kk + 1], rs[:],
                        ALU.mult, ALU.mult, [("yb", h, jj), "par", tag + "rs"], [("yc", kk, j)])

        if "conv" not in PARTS:
            return
        P.barrier()
        R.reset()
        wc = load_w("wc", 1280, 2048)
        zb = R.alloc("zb", [128, 2, T], F32)
        zc = R.alloc("zc", [128, 2, T], F32)
        sqs = [R.alloc("csq", [128, 2, TT], BF16) for _ in range(2)]
        rss = [R.alloc("crs", [128, TT], F32) for _ in range(2)]
        for c in range(2):
            for j in range(NTT):
                b = nextps()
                proj_fm(wc, "wc", 256 + c * 128, j, b)
                act(zb[:, c, tsl(j)], psum[b][:], AF.Copy, [("ps", b)], [("zb", c, j)])
                b = nextps()
                proj_fm(wc, "wc", 512 + c * 128, j, b)
                tt("dve", zb[:, c, tsl(j)], psum[b][:], zb[:, c, tsl(j)], ALU.mult, [("ps", b), ("zb", c, j)], [("zb", c, j)])
            zall = [("zb", c, j) for j in range(NTT)]
            w0 = par[:, C_CW + c * 3 + 0:C_CW + c * 3 + 1]
            w1 = par[:, C_CW + c * 3 + 1:C_CW + c * 3 + 2]
            w2 = par[:, C_CW + c * 3 + 2:C_CW + c * 3 + 3]
            act(zc[:, c, :], zb[:, c, :], AF.Copy, zall + ["par"], [("zc", c)], scale=w2)
            stt("dve", zc[:, c, 1:T], zb[:, c, 0:T - 1], w1, zc[:, c, 1:T], ALU.mult, ALU.add, zall + ["par", ("zc", c)], [("zc", c)])
            stt("dve", zc[:, c, 2:T], zb[:, c, 0:T - 2], w0, zc[:, c, 2:T], ALU.mult, ALU.add, zall + ["par", ("zc", c)], [("zc", c)])
            for j in range(NTT):
                b = nextps()
                proj_fm(wc, "wc", c * 128, j, b)
                tt("dve", zc[:, c, tsl(j)], psum[b][:], zc[:, c, tsl(j)], ALU.mult, [("ps", b), ("zc", c)], [("ycv", c, j)])
        for j in range(NTT):
            gnorm(zc, "ycv", 2, C_GG, 6, sqs, rss, j)

        if "pool" not in PARTS:
            return
        P.barrier()
        R.reset()
        wa = load_w("wa", 0, 256)
        a = R.alloc("a", [128, 2, T], F32)
        t1 = R.alloc("t1", [128, T], F32)
        t2 = R.alloc("t2", [128, T], F32)
        dd = R.alloc("dd", [128, 2, T], BF16)
        pwbd = R.alloc("pwbd", [128, 2, 128], BF16)
        t16 = R.alloc("t16", [128, 16], F32)
        sqs = [R.alloc("asq", [128, 2, TT], BF16) for _ in range(2)]
        rss = [R.alloc("ars", [128, TT], F32) for _ in range(2)]
        memset("dve", pwbd[:], 0.0, ["pwbd"])
        for c in range(2):
            dma("pool", pwbd[0:64, c, 0:64], pw_d[l, 2 * c], "pw%da" % c, reads=["pwbd"], writes=[("pwbd", c, 0)])
            dma("pool", pwbd[64:128, c, 64:128], pw_d[l, 2 * c + 1], "pw%db" % c, reads=["pwbd"], writes=[("pwbd", c, 1)])
        for c in range(2):
            for j in range(NTT):
                b = nextps()
                proj_fm(wa, "wa", c * 128, j, b)
                act(a[:, c, tsl(j)], psum[b][:], AF.Copy, [("ps", b)], [("a", c, j)])
        for c in range(2):
            aall = [("a", c, j) for j in range(NTT)]
            ac = a[:, c, :]

            def shadd(dst, src, k, lo, hi, rd, wr, ac=ac):
                P.op("act", lambda e: e.copy(out=dst[lo:hi, 0:k], in_=src[lo:hi, 0:k]), rd + wr, wr)
                tt("dve", dst[lo:hi, k:T], src[lo:hi, k:T], src[lo:hi, 0:T - k], ALU.add, rd + wr, wr)

            shadd(t1, ac, 1, 0, 128, aall, ["t1"])
            if c == 0:
                shadd(t2, t1, 2, 64, 128, ["t1"], ["t2"])
                P.op("act", lambda e: e.copy(out=t2[0:64, :], in_=t1[0:64, :]), ["t1", "t2"], ["t2"])
            else:
                shadd(t2, t1, 2, 0, 128, ["t1"], ["t2"])
                shadd(t1, t2, 4, 0, 128, ["t2"], ["t1"])
                shadd(t2, t1, 8, 64, 128, ["t1"], ["t2"])
                P.op("act", lambda e: e.copy(out=t2[0:64, :], in_=t1[0:64, :]), ["t1", "t2"], ["t2"])
            stt("dve", dd[:, c, :], t2[:], par[:, C_INVW + c:C_INVW + c + 1], ac, ALU.mult, ALU.subtract,
                ["t2", "par"] + aall, [("dd", c)])
            tt("dve", t16[:], t2[:, 0:16], par[:, C_INVC + c * 16:C_INVC + (c + 1) * 16], ALU.mult, ["t2", "par", "t16"], ["t16"])
            tt("dve", dd[:, c, 0:16], t16[:], a[:, c, 0:16], ALU.subtract, ["t16", ("dd", c)] + aall, [("dd", c)])
        for j in range(NTT):
            for c in range(2):
                b = nextps()
                mm(psum[b][:], pwbd[:, c, :], dd[:, c, tsl(j)], True, True,
                   [("pwbd", c, 0), ("pwbd", c, 1), "pwbd", ("dd", c)], [("ps", b)])
                act(a[:, c, tsl(j)], psum[b][:], AF.Copy, [("ps", b), "par", ("dd", 0), ("dd", 1)], [("ya", c, j), ("a", c, j)],
                    scale=par[:, C_PSC + c:C_PSC + c + 1])
            gnorm(a, "ya", 2, C_GG, 0, sqs, rss, j)

        if "wout" not in PARTS:
            return
        P.barrier()
        R.reset()
        wo = R.alloc("wo", [128, KC, D], BF16)
        dma("pool", wo[:], w_out_d[l].rearrange("(k p) n -> p k n", p=128), "wo", writes=["wo"])
        for j in range(NTT):
            for m in range(KC):
                b = nextps()
                for k in range(KC):
                    mm(psum[b][:], wo[:, k, m * 128:(m + 1) * 128], hcT[:, k, tsl(j)], k == 0, k == KC - 1,
                       ["wo", ("yc", k, j)], [("ps", b)])
                tt("dve", xT[:, m, tsl(j)], xT[:, m, tsl(j)], psum[b][:], ALU.add, [("ps", b), ("x", m, j)], [("x", m, j)])

    def ffn_phase(experts, use_hc):
        slots = []
        for s in range(2):
            wg = R.alloc("wg", [128, KC, FGW], BF16)
            wu = R.alloc("wu", [128, KC, FGW], BF16)
            wd = R.alloc("wd", [128, FGC, D], BF16)
            slots.append((wg, wu, wd))
        acts = [R.alloc("act", [128, FGC, TT], BF16) for _ in range(2)]
        sgs = [R.alloc("sg", [128, TT], F32) for _ in range(2)]
        gi = 0
        ai = 0
        si = 0
        for (wg_d, wu_d, wd_d, e, prep) in experts:
            if prep is not None:
                prep()
            for fg in range(NFG):
                s = gi % 2
                gi += 1
                wg, wu, wd = slots[s]
                c0 = fg * FGW
                dma("pool", wg[:], wg_d.rearrange("(k p) n -> p k n", p=128)[:, :, c0:c0 + FGW], "wg%d" % s, writes=[("wg", s)])
                dma("pool", wu[:], wu_d.rearrange("(k p) n -> p k n", p=128)[:, :, c0:c0 + FGW], "wu%d" % s, writes=[("wu", s)])
                dma("pool", wd[:], wd_d[c0:c0 + FGW, :].rearrange("(f p) n -> p f n", p=128), "wd%d" % s, writes=[("wd", s)])
                for j in range(NTT):
                    at = acts[ai % 2]
                    atag = ("act", ai % 2)
                    ai += 1
                    for f in range(FGC):
                        bg = nextps()
                        for k in range(KC):
                            mm(psum[bg][:], wg[:, k, f * 128:(f + 1) * 128], hT[:, k, tsl(j)], k == 0, k == KC - 1,
                               [("wg", s), ("h", k, j)], [("ps", bg)])
                        bu = nextps()
                        src = hcT if use_hc else hT
                        stag = "hc" if use_hc else "h"
                        for k in range(KC):
                            mm(psum[bu][:], wu[:, k, f * 128:(f + 1) * 128], src[:, k, tsl(j)], k == 0, k == KC - 1,
                               [("wu", s), (stag, k, j)], [("ps", bu)])
                        sg = sgs[si % 2]
                        sgt = ("sg", si % 2)
                        si += 1
                        act(sg[:], psum[bg][:], AF.Silu, [("ps", bg)], [sgt])
                        tt("dve", at[:, f, :], sg[:], psum[bu][:], ALU.mult, [sgt, ("ps", bu)], [(atag, f)])
                    for m in range(KC):
                        b = nextps()
                        for f in range(FGC):
                            mm(psum[b][:], wd[:, f, m * 128:(m + 1) * 128], at[:, f, :], f == 0, f == FGC - 1,
                               [("wd", s), (atag, f)], [("ps", b)])
                        tt("dve", xT[:, m, tsl(j)], xT[:, m, tsl(j)], psum[b][:], ALU.add, [("ps", b), ("x", m, j)], [("x", m, j)])

    def dense_ffn():
        R.reset()
        P.barrier()
        ffn_phase([(fwg_d, fwu_d, fwd_d, 0, None)], False)

    def moe_router_hook(kind, j=None, rs=None, tag=None, extra=None):
        if kind == "alloc":
            h2f = [R.alloc("h2f", [128, KC, TT], F32) for _ in range(1)]
            return h2f
        h2f = extra[0]
        pb = PL
        gcol = pb + 24
        for k in range(KC):
            stt("dve", h2f[:, k, :], xT[:, k, tsl(j)], par[:, gcol + k:gcol + k + 1], rs[:], ALU.mult, ALU.mult,
                [("x", k, j), "par", tag + "rs"], [("h2f", k)])
        for n in range(4):
            b = nextps()
            for k in range(KC):
                mm(psum[b][:, 0:NE], h2f[:, k, n * 128:(n + 1) * 128], rw[:, k, :], k == 0, k == KC - 1,
                   [("h2f", k), "rw"], [("ps", b)])
            tt("dve", comb[:, j * 4 + n, :], psum[b][:, 0:NE], par[:, C_RB:C_RB + NE], ALU.add, [("ps", b), "par"], [("lg", j * 4 + n)])
        for k in range(KC):
            P.op("act", lambda e, k=k: e.copy(out=hT[:, k, tsl(j)], in_=h2f[:, k, :]), [("h2f", k)], [("h", k, j)])
        if ROUTED:
            for n in range(4):
                for kq in range(2):
                    b = nextps()
                    for kk in range(4):
                        k = kq * 4 + kk
                        P.op("pe", lambda e, b=b, kk=kk, k=k, n=n: e.transpose(
                            out=psum[b][:, kk * 128:(kk + 1) * 128], in_=h2f[:, k, n * 128:(n + 1) * 128], identity=ident[:]),
                            [("h2f", k), "ident"], [("ps", b)])
                    P.op("act", lambda e, b=b, n=n, kq=kq: e.copy(out=hTok[:, j * 4 + n, kq * 512:(kq + 1) * 512], in_=psum[b][:]),
                         [("ps", b)], [("htok", j * 4 + n, kq)])

    CAP = 640
    NCH = CAP // 128
    CH = CAP // 2

    def moe_gating():
        P.barrier()
        R.reset()
        lgall = [("lg", i) for i in range(16)]
        m1 = R.alloc("m1", [128, 16], F32)
        m2 = R.alloc("m2", [128, 16], F32)
        eq1 = R.alloc("eq1", [128, 16, NE], F32)
        eq2 = R.alloc("eq2", [128, 16, NE], F32)
        lg2 = R.alloc("lg2", [128, 16, NE], F32)
        g1 = R.alloc("g1", [128, 16], F32)
        g2 = R.alloc("g2", [128, 16], F32)
        ind = R.alloc("ind", [128, 16, NE], F32)
        indb = R.alloc("indb", [128, 16, NE], BF16)
        pos = R.alloc("pos", [128, 16, NE], F32)
        tot = R.alloc("tot", [128, 16, NE], F32)
        boff = R.alloc("boff", [128, 16, NE], F32)
        cnt = R.alloc("cnt", [128, NE], F32)
        maxc = R.alloc("maxc", [128, 1], F32)
        hif = R.alloc("hif", [128, 16, NE], F32)
        ltf = R.alloc("ltf", [128, 128], F32)
        bc3 = lambda t_: t_[:].unsqueeze(2).to_broadcast([128, 16, NE])
        P.op("dve", lambda e: e.tensor_reduce(out=m1[:], in_=comb[:], axis=AX.X, op=ALU.max), lgall, ["m1"])
        tt("dve", eq1[:], comb[:], bc3(m1), ALU.is_equal, lgall + ["m1"], ["eq1"])
        stt("dve", lg2[:], eq1[:], -1e30, comb[:], ALU.mult, ALU.add, ["eq1"] + lgall, ["lg2"])
        P.op("dve", lambda e: e.tensor_reduce(out=m2[:], in_=lg2[:], axis=AX.X, op=ALU.max), ["lg2"], ["m2"])
        tt("dve", eq2[:], lg2[:], bc3(m2), ALU.is_equal, ["lg2", "m2"], ["eq2"])
        tt("dve", ind[:], eq1[:], eq2[:], ALU.add, ["eq1", "eq2"], ["ind"])
        tt("dve", g1[:], m1[:], m2[:], ALU.subtract, ["m1", "m2"], ["g1"])
        act(g1[:], g1[:], AF.Sigmoid, ["g1"], ["g1"])
        ts("dve", g2[:], g1[:], -1.0, 1.0, ALU.mult, ALU.add, ["g1"], ["g2"])
        tt("dve", eq1[:], eq1[:], bc3(g1), ALU.mult, ["eq1", "g1", "ind"], ["eq1"])
        tt("dve", eq2[:], eq2[:], bc3(g2), ALU.mult, ["eq2", "g2", "ind"], ["eq2"])
        tt("dve", comb[:], eq1[:], eq2[:], ALU.add, ["eq1", "eq2"] + lgall, ["comb"])
        if not ROUTED:
            memset("dve", flagf[:], 1.0, ["flagf"])
            P.op("dve", lambda e: e.tensor_copy(out=flagi[:], in_=flagf[:]), ["flagf"], ["flagi"])
            return
        memset("pool", ltf[:], 1.0, ["ltf"])
        P.op("pool", lambda e: e.affine_select(out=ltf[:], in_=ltf[:], compare_op=ALU.is_gt, fill=0.0,
                                               base=0, pattern=[[1, 128]], channel_multiplier=-1), ["ltf"], ["ltf"])
        P.op("act", lambda e: e.copy(out=ltri_bf[:], in_=ltf[:]), ["ltf"], ["ltri"])
        P.op("act", lambda e: e.copy(out=indb[:], in_=ind[:]), ["ind"], ["indb"])
        flat = lambda t_: t_[:].rearrange("p a b -> p (a b)")
        b = nextps()
        mm(psum[b][:, 0:128], ltri_bf[:], flat(indb), True, True, ["ltri", "indb"], [("ps", b)])
        P.op("dve", lambda e, b=b: e.tensor_copy(out=flat(pos), in_=psum[b][:, 0:128]), [("ps", b)], ["pos"])
        b = nextps()
        mm(psum[b][:, 0:128], ones_bf[:], flat(indb), True, True, ["ones_bf", "indb"], [("ps", b)])
        P.op("act", lambda e, b=b: e.copy(out=flat(tot), in_=psum[b][:, 0:128]), [("ps", b)], ["tot"])
        memset("dve", boff[:, 0, :], 0.0, ["boff"])
        for blk in range(1, 16):
            tt("dve", boff[:, blk, :], boff[:, blk - 1, :], tot[:, blk - 1, :], ALU.add, ["boff", "tot"], ["boff"])
        tt("dve", pos[:], pos[:], boff[:], ALU.add, ["pos", "boff"], ["pos"])
        tt("dve", cnt[:], boff[:, 15, :], tot[:, 15, :], ALU.add, ["boff", "tot"], ["cnt"])
        P.op("dve", lambda e: e.tensor_reduce(out=maxc[:], in_=cnt[:], axis=AX.X, op=ALU.max), ["cnt"], ["maxc"])
        ts("dve", flagf[:], maxc[:], float(CAP), 0.0, ALU.subtract, ALU.max, ["maxc"], ["flagf"])
        if FORCE_DENSE:
            ts("dve", flagf[:], flagf[:], 1.0, None, ALU.add, None, ["flagf"], ["flagf"])
        P.op("dve", lambda e: e.tensor_copy(out=flagi[:], in_=flagf[:]), ["flagf"], ["flagi"])
        ts("dve", pos[:], pos[:], 1.0, None, ALU.add, None, ["pos"], ["pos"])
        tt("dve", pos[:], pos[:], ind[:], ALU.mult, ["pos", "ind"], ["pos"])
        ts("dve", posm[:], pos[:], -1.0, None, ALU.add, None, ["pos"], ["posm"])
        P.op("dve", lambda e: e.tensor_copy(out=ghl[:, :, :, 0], in_=comb[:]), ["comb"], ["ghl0"])
        P.op("dve", lambda e: e.tensor_copy(out=hif[:], in_=ghl[:, :, :, 0]), ["ghl0"], ["hif"])
        tt("dve", ghl[:, :, :, 1], comb[:], hif[:], ALU.subtract, ["comb", "hif"], ["ghl1"])

    def moe_routed():
        Rr = Region(nc, hT_off, SB_END - hT_off)
        iota_row = Rr.alloc("iota_row", [128, CAP], F32)
        sid = Rr.alloc("sid", [128, 8], F32)
        sid0 = Rr.alloc("sid0", [128, 1], F32)
        diag = Rr.alloc("diag", [128, 128], F32)
        reps = [Rr.alloc("rep", [128, 128], F32) for _ in range(2)]
        g2s = Rr.alloc("g2s", [128, NCH, 2], F32)
        gsl = [Rr.alloc("gsl", [128, 8], F32) for _ in range(2)]
        slots = []
        for s_ in range(2):
            slots.append((Rr.alloc("rwg", [128, KC, 256], BF16), Rr.alloc("rwu", [128, KC, 256], BF16),
                          Rr.alloc("rwd", [128, 2, D], BF16)))
        acts = [Rr.alloc("ract", [128, 2, CAP], BF16) for _ in range(2)]
        sgs = [Rr.alloc("rsg", [128, CH], F32) for _ in range(2)]
        S = Rr.alloc("S", [128, 16, CAP], BF16)
        STs = [Rr.alloc("ST", [128, NCH, TT], BF16) for _ in range(2)]
        Xe = Rr.alloc("Xe", [128, KC, CAP], BF16)
        Ye = Rr.alloc("Ye", [128, NCH, D], F32)
        Yeb = Rr.alloc("Yeb", [128, NCH, D], BF16)
        b = nextps()
        mm(psum[b][:, 0:1], ltri_bf[:], ones_bf[:, 0:1], True, True, [], [("ps", b)])
        P.op("dve", lambda e, b=b: e.tensor_copy(out=sid0[:], in_=psum[b][:, 0:1]), [("ps", b)], ["sid0"])
        for c in range(NCH):
            ts("dve", sid[:, c:c + 1], sid0[:], float(128 * c), None, ALU.add, None, ["sid0"], [("sid", c)])
        ts("dve", diag[:], ident[:], sid0[:], None, ALU.mult, None, ["sid0"], ["diag"])
        b = nextps()
        mm(psum[b][:, 0:128], ones_f[:], diag[:], True, True, ["diag"], [("ps", b)])
        for c in range(NCH):
            ts("dve", iota_row[:, c * 128:(c + 1) * 128], psum[b][:, 0:128], float(128 * c), None, ALU.add, None,
               [("ps", b)], ["iota"])
        gi = si = sti = ri = 0
        NFG2 = DFF // 256
        for e in range(NE_RUN):
            wg_d = mwg_d[e].rearrange("(k p) n -> p k n", p=128)
            wu_d = mwu_d[e].rearrange("(k p) n -> p k n", p=128)
            for blk in range(16):
                ts("dve", S[:, blk, :], iota_row[:], posm[:, blk, e:e + 1], None, ALU.is_equal, None, ["iota"], [("S", blk)])
            for m in range(KC):
                for half in range(2):
                    b = nextps()
                    for blk in range(16):
                        mm(psum[b][:, 0:CH], hTok[:, blk, m * 128:(m + 1) * 128], S[:, blk, half * CH:(half + 1) * CH],
                           blk == 0, blk == 15, [("S", blk)], [("ps", b)])
                    P.op("act", lambda e_, b=b, m=m, half=half: e_.copy(out=Xe[:, m, half * CH:(half + 1) * CH], in_=psum[b][:, 0:CH]),
                         [("ps", b)], [("Xe", m, half)])
            gs = gsl[e % 2]
            gst = ("gsl", e % 2)
            b = nextps()
            for c in range(NCH):
                for blk in range(16):
                    mm(psum[b][:, 2 * c:2 * c + 2], S[:, blk, c * 128:(c + 1) * 128], ghl[:, blk, e, :], blk == 0, blk == 15,
                       [("S", blk)], [("ps", b)])
            P.op("act", lambda e_, b=b: e_.copy(out=g2s[:].rearrange("p c t -> p (c t)"), in_=psum[b][:, 0:2 * NCH]), [("ps", b)], ["g2s"])
            tt("dve", gs[:, 0:NCH], g2s[:, :, 0], g2s[:, :, 1], ALU.add, ["g2s"], [gst])
            for fg in range(NFG2):
                s_ = gi % 2
                at = acts[gi % 2]
                atag = ("ract", gi % 2)
                gi += 1
                wg, wu, wd = slots[s_]
                c0 = fg * 256
                dma("pool", wg[:], wg_d[:, :, c0:c0 + 256], "rwg%d" % s_, writes=[("rwg", s_)])
                dma("pool", wu[:], wu_d[:, :, c0:c0 + 256], "rwu%d" % s_, writes=[("rwu", s_)])
                dma("pool", wd[:], mwd_d[e][c0:c0 + 256, :].rearrange("(f p) n -> p f n", p=128), "rwd%d" % s_, writes=[("rwd", s_)])
                for f in range(2):
                    for half in range(2):
                        hs = slice(half * CH, (half + 1) * CH)
                        bg = nextps()
                        for k in range(KC):
                            mm(psum[bg][:, 0:CH], wg[:, k, f * 128:(f + 1) * 128], Xe[:, k, hs], k == 0, k == KC - 1,
                               [("rwg", s_), ("Xe", k, half)], [("ps", bg)])
                        bu = nextps()
                        for k in range(KC):
                            mm(psum[bu][:, 0:CH], wu[:, k, f * 128:(f + 1) * 128], Xe[:, k, hs], k == 0, k == KC - 1,
                               [("rwu", s_), ("Xe", k, half)], [("ps", bu)])
                        sg = sgs[si % 2]
                        sgt = ("rsg", si % 2)
                        si += 1
                        act(sg[:], psum[bg][:, 0:CH], AF.Silu, [("ps", bg)], [sgt])
                        tt("dve", at[:, f, hs], sg[:], psum[bu][:, 0:CH], ALU.mult, [sgt, ("ps", bu)], [(atag, f, half)])
                for c in range(NCH):
                    for fh in range(2):
                        b = nextps()
                        for f in range(2):
                            mm(psum[b][:], at[:, f, c * 128:(c + 1) * 128], wd[:, f, fh * 512:(fh + 1) * 512], f == 0, f == 1,
                               [("rwd", s_), (atag, f, 0), (atag, f, 1)], [("ps", b)])
                        ysl = Ye[:, c, fh * 512:(fh + 1) * 512]
                        ytag = ("Ye", c, fh)
                        gcol = gs[:, c:c + 1]
                        if fg == 0:
                            ts("dve", ysl, psum[b][:], gcol, None, ALU.mult, None, [("ps", b), gst], [ytag])
                        elif fg < NFG2 - 1:
                            stt("dve", ysl, psum[b][:], gcol, ysl, ALU.mult, ALU.add, [("ps", b), gst, ytag], [ytag])
                        else:
                            stt("dve", Yeb[:, c, fh * 512:(fh + 1) * 512], psum[b][:], gcol, ysl, ALU.mult, ALU.add,
                                [("ps", b), gst, ytag], [("Yeb", c, fh)])
            for n in range(NTT):
                STn = STs[sti % 2]
                stt_ = ("ST", sti % 2)
                sti += 1
                bp = nextps()
                for bl in range(4):
                    blk = n * 4 + bl
                    rp = reps[ri % 2]
                    rpt = ("rep", ri % 2)
                    ri += 1
                    P.op("dve", lambda e_, rp=rp, blk=blk, e=e: e_.tensor_copy(out=rp[:], in_=posm[:, blk, e:e + 1].to_broadcast([128, 128])),
                         [], [rpt])
                    mm(psum[bp][:, bl * 128:(bl + 1) * 128], rp[:], ident[:], True, True, [rpt], [("ps", bp)])
                for c in range(NCH):
                    ts("dve", STn[:, c, :], psum[bp][:], sid[:, c:c + 1], None, ALU.is_equal, None, [("ps", bp), ("sid", c)], [(stt_, c)])
                for m in range(KC):
                    b = nextps()
                    for c in range(NCH):
                        mm(psum[b][:], Yeb[:, c, m * 128:(m + 1) * 128], STn[:, c, :], c == 0, c == NCH - 1,
                           [("Yeb", c, m // 4), (stt_, c)], [("ps", b)])
                    tt("dve", xT[:, m, tsl(n)], xT[:, m, tsl(n)], psum[b][:], ALU.add, [("ps", b), ("x", m, n)], [("x", m, n)])

    def moe():
        R.reset()
        R.reset()
        ffn_bytes = 2 * (2 * KC * FGW * 2 + FGC * D * 2) + 2 * (FGC * TT * 2) + 2 * (TT * 4)
        tail = Region(nc, R.base + ffn_bytes, R.size - ffn_bytes - 2048)
        combT = tail.alloc("combT", [NE, T], F32)
        sel = tail.alloc("sel", [NE, NE, 128], F32)
        for e in range(NE):
            ts("dve", sel[:, e, :], ones_f[0:NE, :], ident[0:NE, e:e + 1], None, ALU.mult, None, ["ones_f", "ident"], [("sel", e)])
        for j in range(NTT):
            b = nextps()
            for n in range(4):
                P.op("pe", lambda e, b=b, n=n, j=j: e.transpose(out=psum[b][0:NE, n * 128:(n + 1) * 128],
                                                               in_=comb[:, j * 4 + n, :], identity=ident[:]),
                     ["comb", "ident"], [("ps", b)])
            P.op("act", lambda e, b=b, j=j: e.copy(out=combT[:, tsl(j)], in_=psum[b][0:NE, :]), [("ps", b)], [("combT", j)])

        def make_prep(e):
            def prep():
                for j in range(NTT):
                    b = nextps()
                    mm(psum[b][:], sel[:, e, :], combT[:, tsl(j)], True, True, [("sel", e), ("combT", j)], [("ps", b)])
                    for k in range(KC):
                        tt("dve", hcT[:, k, tsl(j)], hT[:, k, tsl(j)], psum[b][:], ALU.mult,
                           [("h", k, j), ("ps", b)], [("hc", k, j)])
            return prep

        experts = [(mwg_d[e], mwu_d[e], mwd_d[e], e, make_prep(e)) for e in range(NE_RUN)]
        ffn_phase(experts, True)

    out_idx = []

    def final_norm(raw=False):
        P.barrier()
        R.reset()
        sqs = [R.alloc("fsq", [128, KC, TT], BF16) for _ in range(2)]
        rss = [R.alloc("frs", [128, TT], F32) for _ in range(2)]
        obs = [R.alloc("fo", [128, KC, TT], F32) for _ in range(2)]
        outv = outT_d.rearrange("(k p) t -> p k t", p=128)
        for j in range(NTT):
            ob = obs[j % 2]
            otag = ("fo", j % 2)
            if raw:
                for k in range(KC):
                    P.op("act", lambda e, k=k, j=j, ob=ob: e.copy(out=ob[:, k, :], in_=xT[:, k, tsl(j)]), [("x", k, j)], [(otag, k)])
            else:
                sq, rs = sqs[j % 2], rss[j % 2]
                tag = "f%d" % (j % 2)
                group_rstd([(xT[:, k, tsl(j)], ("x", k, j)) for k in range(KC)], None, D, sq, rs, tag)
                for k in range(KC):
                    stt("dve", ob[:, k, :], xT[:, k, tsl(j)], par[:, C_FG + k:C_FG + k + 1], rs[:], ALU.mult, ALU.mult,
                        [("x", k, j), "par", tag + "rs"], [(otag, k)])
            out_idx.append(dma("sp", outv[:, :, tsl(j)], ob[:], "out%d" % (j % 2), reads=[(otag, k) for k in range(KC)]))

    rmsnorm_full(0 * PL + 0, hT, "h")
    mixer(0)
    if stage >= 2:
        rmsnorm_full(0 * PL + 24, hT, "h")
        dense_ffn()
    if stage >= 3:
        rmsnorm_full(1 * PL + 0, hT, "h")
        mixer(1)
    if stage >= 4:
        rmsnorm_full(1 * PL + 24, None, "h", hook=moe_router_hook)
        moe_gating()
        P.segment(1)
        if DEBUG:
            memset("dve", dbg[:, 1:2], 1.0, ["dbgm"])
        if ROUTED:
            moe_routed()
        P.segment(2)
        if DEBUG:
            memset("dve", dbg[:, 1:2], 2.0, ["dbgm"])
        moe()
        P.segment(3)
        if DEBUG:
            P.op("dve", lambda e: e.tensor_copy(out=dbg[:, 0:1], in_=flagf[:]), [], ["dbgf"])
            out_idx.append(dma("sp", dbg_d, dbg[:], "dbgo", reads=["dbgf"]))
    final_norm(raw=(stage < 99))
    nw = P.emit(final_waits=out_idx, flag_ap=flagi[0:1, 0:1], dummies=dummies)
    return nc, len(P.ops), nw


def host_layout(inputs):
    f = lambda a: np.ascontiguousarray(np.asarray(a, dtype=np.float32))
    g = {k: np.asarray(v) for k, v in inputs.items()}
    par = np.zeros((128, NPAR), np.float32)
    cols = lambda v, n: np.asarray(v, np.float32).reshape(n, 128).T
    for l in range(2):
        pb = l * PL
        par[:, pb:pb + 8] = cols(g["norm1_g"][l], 8)
        par[:, pb + 8:pb + 10] = cols(g["pool_scale"][l], 2)
        cw = np.asarray(g["conv_w"][l], np.float32)
        for c in range(2):
            for tap in range(3):
                par[:, pb + 10 + c * 3 + tap] = cw[tap, c * 128:(c + 1) * 128]
        par[:, pb + 16:pb + 24] = cols(g["group_g"][l], 8)
        par[:, pb + 24:pb + 32] = cols(g["norm2_g"][l], 8)
    par[:, C_FG:C_FG + 8] = cols(g["final_g"], 8)
    par[:, C_RB:C_RB + 8] = np.broadcast_to(np.asarray(g["router_b"][0], np.float32)[None, :], (128, 8))
    wins = np.array([[2, 4], [8, 16]], np.float32)
    for c in range(2):
        wcol = np.repeat(wins[c], 64)
        par[:, C_INVW + c] = 1.0 / wcol
        tpos = np.arange(16, dtype=np.float32)[None, :] + 1.0
        par[:, C_INVC + c * 16:C_INVC + (c + 1) * 16] = 1.0 / np.minimum(tpos, wcol[:, None])
    bc = np.zeros((2, 3, 128, 512), np.float32)
    for l in range(2):
        bc[l, 0] = np.broadcast_to(np.asarray(g["sgu_ln_g"][l], np.float32)[None, :], (128, 512))
        bc[l, 1] = np.broadcast_to(np.asarray(g["sgu_ln_b"][l], np.float32)[None, :], (128, 512))
        bc[l, 2] = np.broadcast_to(np.asarray(g["sgu_b"][l], np.float32).reshape(1, 512), (128, 512))
    shared = {
        "w_in": f(g["w_in"]), "w_out": f(g["w_out"]),
        "ffn_wg": f(g["ffn_w_gate"][0]), "ffn_wu": f(g["ffn_w_up"][0]), "ffn_wd": f(g["ffn_w_down"][0]),
        "moe_wg": f(g["moe_w_gate"][0]), "moe_wu": f(g["moe_w_up"][0]), "moe_wd": f(g["moe_w_down"][0]),
        "router_wl": f(np.asarray(g["router_w"][0], np.float32).reshape(KC, 128, NE).transpose(1, 0, 2)),
        "params": par, "bc": bc,
        "sgu_wT": f(np.asarray(g["sgu_w"], np.float32).transpose(0, 3, 1, 2)),
        "pool_w": f(g["pool_w"]),
    }
    x = np.asarray(g["x"], np.float32)
    in_maps = []
    for b in range(x.shape[0]):
        m = dict(shared)
        m["xT"] = np.ascontiguousarray(x[b].T)
        in_maps.append(m)
    return in_maps


_NC_CACHE = {}


def kernel(**inputs):
    in_maps = host_layout(inputs)
    if 99 not in _NC_CACHE:
        _NC_CACHE[99] = build_nc(99)[0]
    nc = _NC_CACHE[99]
    res = run_bass_kernel_spmd(nc, in_maps, core_ids=list(range(8)))
    out = np.stack([np.ascontiguousarray(r["outT"].T) for r in res.results], axis=0)
    return out.astype(np.float32)
```

```python
import bisect
import numpy as np
import concourse.bass as bass
import concourse.mybir as mybir
from concourse.bass_utils import run_bass_kernel_spmd

F32 = mybir.dt.float32
BF16 = mybir.dt.bfloat16
ALU = mybir.AluOpType
AF = mybir.ActivationFunctionType
AX = mybir.AxisListType

D = 1024
T = 2048
KC = 8
TT = 512
NTT = 4
DIN = 2048
DFF = 3584
FGC = 4
FGW = FGC * 128
NFG = DFF // FGW
NE = 8
EPS = 1e-6
PL = 32
NPAR = 2 * PL + 8 + 8 + 2 + 32
C_FG = 2 * PL
C_RB = C_FG + 8
C_INVW = C_RB + 8
C_INVC = C_INVW + 2
SB_BASE = 16512
SB_END = 229376


class _Op:
    __slots__ = ("eng", "fn", "deps", "dma", "signal", "sigval", "idx", "seg")


class Prog:
    def __init__(self, nc):
        self.nc = nc
        self.ops = []
        self.last_writer = {}
        self.readers = {}
        self.last_on_eng = {}
        self.pending_bar = {}
        self.dangling_dma = set()
        self.seg = 0
        self.seg_end = {}

    def segment(self, seg):
        self.seg_end[self.seg] = set(self.last_on_eng.values()) | set(self.dangling_dma)
        self.seg = seg
        self.last_writer = {}
        self.readers = {}
        self.last_on_eng = {}
        self.pending_bar = {}
        self.dangling_dma = set()

    def op(self, eng, fn, reads=(), writes=(), dma=None):
        o = _Op()
        o.eng, o.fn, o.dma = eng, fn, dma
        o.seg = self.seg
        o.signal = dma is not None
        o.sigval = None
        o.idx = len(self.ops)
        writes = list(writes) + [r for r in reads if isinstance(r, tuple) and r[0] == "ps" and r not in writes]
        deps = set()
        for r in reads:
            w = self.last_writer.get(r)
            if w is not None:
                deps.add(w)
        for w_ in writes:
            w = self.last_writer.get(w_)
            if w is not None:
                deps.add(w)
            for rd in self.readers.get(w_, ()):
                deps.add(rd)
        pb = self.pending_bar.pop(eng, None)
        if pb:
            deps |= pb
        deps.discard(o.idx)
        o.deps = deps
        for d in deps:
            self.dangling_dma.discard(d)
        for r in reads:
            self.readers.setdefault(r, []).append(o.idx)
        for w_ in writes:
            self.last_writer[w_] = o.idx
            self.readers[w_] = []
        if dma is not None:
            self.dangling_dma.add(o.idx)
        else:
            self.last_on_eng[eng] = o.idx
        self.ops.append(o)
        return o.idx

    def barrier(self):
        deps = set(self.last_on_eng.values()) | set(self.dangling_dma)
        for e in ("pe", "act", "dve", "pool", "sp"):
            self.pending_bar[e] = set(deps) | self.pending_bar.get(e, set())
        self.dangling_dma = set()

    def emit(self, final_waits=(), flag_ap=None, dummies=None):
        nc = self.nc
        ops = self.ops
        for idx in final_waits:
            ops[idx].signal = True
        for sg_, deps_ in self.seg_end.items():
            for d in deps_:
                ops[d].signal = True
        for o in ops:
            nd = set()
            for d in o.deps:
                p = ops[d]
                assert p.seg == o.seg
                if p.dma is None and o.dma is None and p.eng == "pe" and o.eng == "pe":
                    continue
                nd.add(d)
            o.deps = nd
            for d in nd:
                ops[d].signal = True
        counters = {}
        semkeys = []
        dma_hist = {}
        for o in ops:
            if not o.signal:
                continue
            key = (o.seg, "dma", o.dma) if o.dma is not None else (o.seg, "eng", o.eng)
            if key not in counters:
                counters[key] = 0
                semkeys.append(key)
            counters[key] += 16 if o.dma is not None else 1
            o.sigval = (key, counters[key])
            if o.dma is not None:
                dma_hist.setdefault(key, ([], []))
                dma_hist[key][0].append(o.idx)
                dma_hist[key][1].append(counters[key])
        sems = {}
        for key in semkeys:
            sems[key] = nc.alloc_semaphore("s%d_%s_%s" % key)
        engobj = ("sp", "pool", "act", "dve", "pe")
        per_eng = {e: {0: [], 1: [], 2: [], 3: []} for e in engobj}
        for o in ops:
            per_eng[o.eng][o.seg].append(o)
        branching = any(o.seg in (1, 2) for o in ops)
        bar0 = nc.alloc_semaphore("bar0") if branching else None
        bar1 = nc.alloc_semaphore("bar1") if branching else None
        nwaits = [0]

        def waitval(consumer_idx, d):
            key, val = ops[d].sigval
            if key[1] == "dma":
                idxs, vals = dma_hist[key]
                pos = bisect.bisect_left(idxs, consumer_idx) - 1
                val = max(val, vals[pos])
            return key, val

        def emit_ops(eng, lst):
            waited = {}
            for o in lst:
                need = {}
                for d in o.deps:
                    key, val = waitval(o.idx, d)
                    if need.get(key, 0) < val:
                        need[key] = val
                for key, val in need.items():
                    if waited.get(key, 0) >= val:
                        continue
                    eng.wait_ge(sems[key], val)
                    waited[key] = val
                    nwaits[0] += 1
                ins = o.fn(eng)
                if o.signal:
                    ins.then_inc(sems[o.sigval[0]], 16 if o.dma is not None else 1)

        def wait_seg_end(eng, seg):
            need = {}
            for d in self.seg_end.get(seg, ()):
                key, val = ops[d].sigval
                if key[1] == "dma":
                    val = max(val, dma_hist[key][1][-1] if False else val)
                if need.get(key, 0) < val:
                    need[key] = val
            for key, val in need.items():
                eng.wait_ge(sems[key], val)
                nwaits[0] += 1

        def end_seg(eng, ename, bsem):
            if ename == "act":
                eng.copy(out=dummies["act"][:, 0:1], in_=dummies["act"][:, 1:2]).then_inc(bsem, 1)
            elif ename == "dve":
                eng.tensor_copy(out=dummies["dve"][:, 0:1], in_=dummies["dve"][:, 1:2]).then_inc(bsem, 1)
            elif ename == "pool":
                eng.memset(dummies["pool"][:, 0:1], 0.0).then_inc(bsem, 1)

        with nc.Block() as block:
            deco = {"pe": block.tensor, "act": block.scalar, "dve": block.vector,
                    "pool": block.gpsimd, "sp": block.sync}
            for ename in engobj:
                segs = per_eng[ename]

                def body(eng, segs=segs, ename=ename):
                    emit_ops(eng, segs[0])
                    if branching:
                        end_seg(eng, ename, bar0)
                        eng.wait_ge(bar0, 3)
                        wait_seg_end(eng, 0)
                        reg = eng.alloc_register("flag_" + ename)
                        eng.reg_load(reg, flag_ap)
                        with eng.If_eq(reg, 0):
                            emit_ops(eng, segs[1])
                            wait_seg_end(eng, 1)
                            end_seg(eng, ename, bar1)
                        with eng.Else():
                            emit_ops(eng, segs[2])
                            wait_seg_end(eng, 2)
                            end_seg(eng, ename, bar1)
                        eng.wait_ge(bar1, 3)
                    emit_ops(eng, segs[3])
                    if ename == "sp":
                        for idx in final_waits:
                            key, val = ops[idx].sigval
                            eng.wait_ge(sems[key], val)

                deco[ename](body)
        return nwaits[0]


class Region:
    def __init__(self, nc, base, size):
        self.nc, self.base, self.size = nc, base, size
        self.off = 0
        self.n = 0

    def reset(self):
        self.off = 0

    def alloc(self, name, shape, dtype):
        esz = 2 if dtype == BF16 else 4
        nbytes = esz
        for s in shape[1:]:
            nbytes *= s
        nbytes = (nbytes + 31) // 32 * 32
        assert self.off + nbytes <= self.size, (name, self.off, nbytes, self.size)
        self.n += 1
        t = self.nc.alloc_sbuf_tensor_at("%s_%d" % (name, self.n), list(shape), dtype,
                                         offset=self.base + self.off)
        self.off += nbytes
        return t


def build_nc(stage=99, PARTS=("sgu", "conv", "pool", "wout"), NE_RUN=NE, ROUTED=True, FORCE_DENSE=False, DEBUG=False):
    nc = bass.Bass("TRN2", target_bir_lowering=False)
    dt = lambda n, s: nc.dram_tensor(n, s, F32, kind="ExternalInput").ap()
    xT_d = dt("xT", [D, T])
    w_in_d = dt("w_in", [2, D, DIN])
    w_out_d = dt("w_out", [2, D, D])
    if stage >= 2:
        fwg_d = dt("ffn_wg", [D, DFF])
        fwu_d = dt("ffn_wu", [D, DFF])
        fwd_d = dt("ffn_wd", [DFF, D])
    if stage >= 4:
        mwg_d = dt("moe_wg", [NE_RUN, D, DFF])
        mwu_d = dt("moe_wu", [NE_RUN, D, DFF])
        mwd_d = dt("moe_wd", [NE_RUN, DFF, D])
    rw_d = dt("router_wl", [128, KC, NE])
    par_d = dt("params", [128, NPAR])
    bc_d = dt("bc", [2, 3, 128, 512])
    swT_d = dt("sgu_wT", [2, 128, 4, 128])
    pw_d = dt("pool_w", [2, 4, 64, 64])
    outT_d = nc.dram_tensor("outT", [D, T], F32, kind="ExternalOutput").ap()
    dbg_d = nc.dram_tensor("dbg", [128, 2], F32, kind="ExternalOutput").ap() if DEBUG else None

    pers = Region(nc, SB_BASE, SB_END - SB_BASE)
    xT = pers.alloc("xT", [128, KC, T], F32)
    hcT_off = SB_BASE + pers.off
    hcT = pers.alloc("hcT", [128, KC, T], BF16)
    hTok = nc.alloc_sbuf_tensor_at("hTok", [128, 16, D], BF16, offset=hcT_off)
    par = pers.alloc("par", [128, NPAR], F32)
    ident = pers.alloc("ident", [128, 128], F32)
    ones_bf = pers.alloc("ones_bf", [128, 128], BF16)
    ones_f = pers.alloc("ones_f", [128, 128], F32)
    rw = pers.alloc("rw", [128, KC, NE], F32)
    comb = pers.alloc("comb", [128, 16, NE], F32)
    posm = pers.alloc("posm", [128, 16, NE], F32)
    ghl = pers.alloc("ghl", [128, 16, NE, 2], BF16)
    ltri_bf = pers.alloc("ltri_bf", [128, 128], BF16)
    flagf = pers.alloc("flagf", [128, 1], F32)
    flagi = pers.alloc("flagi", [128, 1], mybir.dt.int32)
    dummies = {"act": pers.alloc("dumA", [128, 2], F32), "dve": pers.alloc("dumD", [128, 2], F32),
               "pool": pers.alloc("dumP", [128, 2], F32)}
    dbg = pers.alloc("dbg", [128, 2], F32)
    hT_off = SB_BASE + pers.off
    hT = pers.alloc("hT", [128, KC, T], BF16)
    R = Region(nc, SB_BASE + pers.off, SB_END - SB_BASE - pers.off)
    psum = [nc.alloc_psum_tensor("ps%d" % i, [128, 512], F32) for i in range(8)]

    P = Prog(nc)
    psc = [0]

    def nextps():
        b = psc[0] % 8
        psc[0] += 1
        return b

    def tsl(j):
        return slice(j * TT, (j + 1) * TT)

    def mm(out, lhsT, rhs, start, stop, reads, writes):
        P.op("pe", lambda e: e.matmul(out, lhsT=lhsT, rhs=rhs, start=start, stop=stop), reads, writes)

    def dma(q, out, in_, key, reads=(), writes=()):
        return P.op(q, lambda e: e.dma_start(out=out, in_=in_), reads, writes, dma=key)

    def tt(eng, out, in0, in1, op, reads, writes):
        P.op(eng, lambda e: e.tensor_tensor(out=out, in0=in0, in1=in1, op=op), reads, writes)

    def stt(eng, out, in0, scalar, in1, op0, op1, reads, writes):
        P.op(eng, lambda e: e.scalar_tensor_tensor(out=out, in0=in0, scalar=scalar, in1=in1, op0=op0, op1=op1),
             reads, writes)

    def ts(eng, out, in0, s1, s2, op0, op1, reads, writes):
        if s2 is None:
            P.op(eng, lambda e: e.tensor_scalar(out=out, in0=in0, scalar1=s1, scalar2=None, op0=op0), reads, writes)
        else:
            P.op(eng, lambda e: e.tensor_scalar(out=out, in0=in0, scalar1=s1, scalar2=s2, op0=op0, op1=op1),
                 reads, writes)

    def act(out, in_, func, reads, writes, bias=None, scale=None, accum_out=None):
        kw = {}
        if bias is not None:
            kw["bias"] = bias
        if scale is not None:
            kw["scale"] = scale
        if accum_out is not None:
            kw["accum_out"] = accum_out
        P.op("act", lambda e: e.activation(out=out, in_=in_, func=func, **kw), reads, writes)

    def recip(out, in_, reads, writes):
        P.op("dve", lambda e: e.reciprocal(out=out, in_=in_), reads, writes)

    def memset(eng, ap, val, writes):
        P.op(eng, lambda e: e.memset(ap, val), (), writes)

    dma("sp", xT[:], xT_d.rearrange("(k p) t -> p k t", p=128), "xT", writes=[("x", k, j) for k in range(KC) for j in range(NTT)])
    dma("sp", par[:], par_d, "par", writes=["par"])
    dma("sp", rw[:], rw_d, "rw", writes=["rw"])
    memset("pool", ident[:], 0.0, ["ident"])
    P.op("pool", lambda e: e.affine_select(out=ident[:], in_=ident[:], compare_op=ALU.not_equal, fill=1.0,
                                           base=0, pattern=[[-1, 128]], channel_multiplier=1),
         ["ident"], ["ident"])
    memset("pool", ones_bf[:], 1.0, ["ones_bf"])
    memset("pool", ones_f[:], 1.0, ["ones_f"])
    for dn in ("act", "dve", "pool"):
        memset("pool", dummies[dn][:], 0.0, ["dum" + dn])

    def group_rstd(srcs, src_tokens, nchan, sq, rs, tag):
        n = len(srcs)
        for i, (ap, tok) in enumerate(srcs):
            act(sq[:, i, :], ap, AF.Square, [tok], [(tag + "sq", i)])
        b = nextps()
        for i in range(n):
            mm(psum[b][:], ones_bf[:], sq[:, i, :], i == 0, i == n - 1, [(tag + "sq", i), "ones_bf"], [("ps", b)])
        act(rs[:], psum[b][:], AF.Sqrt, [("ps", b)], [tag + "rs"], bias=EPS, scale=1.0 / nchan)
        recip(rs[:], rs[:], [tag + "rs"], [tag + "rs"])

    def rmsnorm_full(gcol, dst, dst_tag, hook=None):
        P.barrier()
        R.reset()
        sqs = [R.alloc("nsq", [128, KC, TT], BF16) for _ in range(2)]
        rss = [R.alloc("nrs", [128, TT], F32) for _ in range(2)]
        extra = hook("alloc") if hook else None
        for j in range(NTT):
            sq, rs = sqs[j % 2], rss[j % 2]
            tag = "n%d" % (j % 2)
            group_rstd([(xT[:, k, tsl(j)], ("x", k, j)) for k in range(KC)], None, D, sq, rs, tag)
            if hook:
                hook("tile", j, rs, tag, extra)
            if dst is not None:
                for k in range(KC):
                    stt("dve", dst[:, k, tsl(j)], xT[:, k, tsl(j)], par[:, gcol + k:gcol + k + 1], rs[:],
                        ALU.mult, ALU.mult, [("x", k, j), "par", tag + "rs"], [(dst_tag, k, j)])

    def mixer(l):
        pb = l * PL
        C_N1, C_PSC, C_CW, C_GG = pb, pb + 8, pb + 10, pb + 16
        w_in_l = w_in_d[l].rearrange("(k p) n -> p k n", p=128)

        def load_w(name, c0, c1):
            wt = R.alloc(name, [128, KC, c1 - c0], BF16)
            dma("pool", wt[:], w_in_l[:, :, c0:c1], name, writes=[name])
            return wt

        def proj_fm(wt, name, col0, j, b):
            for k in range(KC):
                mm(psum[b][:], wt[:, k, col0:col0 + 128], hT[:, k, tsl(j)], k == 0, k == KC - 1,
                   [name, ("h", k, j)], [("ps", b)])

        def gnorm(ybuf, ytag, nch, gbase, ycat_c0, sqs, rss, j):
            sq, rs = sqs[j % 2], rss[j % 2]
            tag = ytag + "g%d" % (j % 2)
            group_rstd([(ybuf[:, c, tsl(j)], (ytag, c, j)) for c in range(nch)], None, nch * 128, sq, rs, tag)
            for c in range(nch):
                kk = ycat_c0 + c
                stt("dve", hcT[:, kk, tsl(j)], ybuf[:, c, tsl(j)], par[:, C_GG + kk:C_GG + kk + 1], rs[:],
                    ALU.mult, ALU.mult, [(ytag, c, j), "par", tag + "rs"], [("yc", kk, j)])

        for hf in (range(2) if "sgu" in PARTS else ()):
          if hf == 0:
            P.barrier()
            R.reset()
            wv = load_w("wv", 768, 1280)
            wu = load_w("wu", 256, 768)
            lng = R.alloc("lng", [128, 512], F32)
            lnb = R.alloc("lnb", [128, 512], F32)
            sbb = R.alloc("sbb", [128, 512], F32)
            dma("sp", lng[:], bc_d[l, 0], "lng", writes=["lng"])
            dma("sp", lnb[:], bc_d[l, 1], "lnb", writes=["lnb"])
            dma("sp", sbb[:], bc_d[l, 2], "sbb", writes=["sbb"])
            wmT = R.alloc("wmT", [128, 4, 128], BF16)
            dma("pool", wmT[:], swT_d[l], "wmT", writes=["wmT"])
            memset("dve", wmT[64:128, :, 0:64], 0.0, ["wmT"])
            zn = R.alloc("zn", [128, 8, 512], BF16)
            mixb = R.alloc("mixb", [128, 4, 1024], F32)
            vt = [R.alloc("vt", [128, 512], F32) for _ in range(2)]
            junk = R.alloc("junk", [128, 512], BF16)
            st = R.alloc("st", [128, 16, 8], F32)
            memset("dve", st[:], 0.0, ["st"])
            sqs = [R.alloc("bsq", [128, 4, TT], BF16) for _ in range(2)]
            rss = [R.alloc("brs", [128, TT], F32) for _ in range(2)]
          if True:
            for n in range(8):
                tok0 = hf * 1024 + n * 128
                jt = tok0 // TT
                b = nextps()
                for k in range(KC):
                    mm(psum[b][:], hT[:, k, tok0:tok0 + 128], wv[:, k, :], k == 0, k == KC - 1,
                       [("h", k, jt), "wv"], [("ps", b)])
                ns = n + 8 * hf
                s_sum, s_ssq = st[:, ns, 0:1], st[:, ns, 1:2]
                s_mean, s_m2, s_var, s_rstd = st[:, ns, 2:3], st[:, ns, 3:4], st[:, ns, 4:5], st[:, ns, 5:6]
                stn = ("st", ns)
                P.op("dve", lambda e, o=s_sum, i=psum[b][:]: e.tensor_reduce(out=o, in_=i, axis=AX.X, op=ALU.add),
                     [("ps", b), "st"], [(stn, 0)])
                act(junk[:], psum[b][:], AF.Square, [("ps", b), "st"], [(stn, 1), "junk"], accum_out=s_ssq)
                ts("dve", s_mean, s_sum, 1.0 / 512, None, ALU.mult, None, [(stn, 0)], [(stn, 2)])
                tt("dve", s_m2, s_mean, s_mean, ALU.mult, [(stn, 2)], [(stn, 3)])
                stt("dve", s_var, s_ssq, 1.0 / 512, s_m2, ALU.mult, ALU.subtract, [(stn, 1), (stn, 3)], [(stn, 4)])
                act(s_rstd, s_var, AF.Sqrt, [(stn, 4)], [(stn, 5)], bias=EPS)
                recip(s_rstd, s_rstd, [(stn, 5)], [(stn, 5)])
                v = vt[n % 2]
                vtag = ("vt", n % 2)
                ts("dve", v[:], psum[b][:], s_mean, s_rstd, ALU.subtract, ALU.mult,
                   [("ps", b), (stn, 2), (stn, 5)], [vtag])
                tt("pool", v[:], v[:], lng[:], ALU.mult, [vtag, "lng"], [vtag])
                tt("pool", zn[:, n, :], v[:], lnb[:], ALU.add, [vtag, "lnb"], [("zn", n)])
                b2 = nextps()
                for h in range(4):
                    mm(psum[b2][:, h * 128:(h + 1) * 128], zn[:, n, h * 128:(h + 1) * 128], wmT[:, h, :], True, True,
                       [("zn", n), "wmT"], [("ps", b2)])
                tt("dve", mixb[:, :, n * 128:(n + 1) * 128], psum[b2][:].rearrange("p (h i) -> p h i", h=4),
                   sbb[:].rearrange("p (h i) -> p h i", h=4), ALU.add, [("ps", b2), "sbb"],
                   [("mixb", n)] + [("yb", h_, n // 4) for h_ in range(4)])
            for jj in range(2):
                j = hf * 2 + jj
                for h in range(4):
                    b = nextps()
                    proj_fm(wu, "wu", h * 128, j, b)
                    tt("dve", mixb[:, h, jj * TT:(jj + 1) * TT], psum[b][:], mixb[:, h, jj * TT:(jj + 1) * TT], ALU.mult,
                       [("ps", b)] + [("mixb", jj * 4 + q) for q in range(4)], [("yb", h, jj)])
                sq, rs = sqs[jj], rss[jj]
                tag = "gb%d" % jj
                group_rstd([(mixb[:, h, jj * TT:(jj + 1) * TT], ("yb", h, jj)) for h in range(4)], None, 512, sq, rs, tag)
                for h in range(4):
                    kk = 2 + h
                    stt("dve", hcT[:, kk, tsl(j)], mixb[:, h, jj * TT:(jj + 1) * TT], par[:, C_GG + kk:C_GG + kk + 1], rs[:],
                        ALU.mult, ALU.mult, [("yb", h, jj), "par", tag + "rs"], [("yc", kk, j)])

        if "conv" not in PARTS:
            return
        P.barrier()
        R.reset()
        wc = load_w("wc", 1280, 2048)
        zb = R.alloc("zb", [128, 2, T], F32)
        zc = R.alloc("zc", [128, 2, T], F32)
        sqs = [R.alloc("csq", [128, 2, TT], BF16) for _ in range(2)]
        rss = [R.alloc("crs", [128, TT], F32) for _ in range(2)]
        for c in range(2):
            for j in range(NTT):
                b = nextps()
                proj_fm(wc, "wc", 256 + c * 128, j, b)
                act(zb[:, c, tsl(j)], psum[b][:], AF.Copy, [("ps", b)], [("zb", c, j)])
                b = nextps()
                proj_fm(wc, "wc", 512 + c * 128, j, b)
                tt("dve", zb[:, c, tsl(j)], psum[b][:], zb[:, c, tsl(j)], ALU.mult, [("ps", b), ("zb", c, j)], [("zb", c, j)])
            zall = [("zb", c, j) for j in range(NTT)]
            w0 = par[:, C_CW + c * 3 + 0:C_CW + c * 3 + 1]
            w1 = par[:, C_CW + c * 3 + 1:C_CW + c * 3 + 2]
            w2 = par[:, C_CW + c * 3 + 2:C_CW + c * 3 + 3]
            act(zc[:, c, :], zb[:, c, :], AF.Copy, zall + ["par"], [("zc", c)], scale=w2)
            stt("dve", zc[:, c, 1:T], zb[:, c, 0:T - 1], w1, zc[:, c, 1:T], ALU.mult, ALU.add, zall + ["par", ("zc", c)], [("zc", c)])
            stt("dve", zc[:, c, 2:T], zb[:, c, 0:T - 2], w0, zc[:, c, 2:T], ALU.mult, ALU.add, zall + ["par", ("zc", c)], [("zc", c)])
            for j in range(NTT):
                b = nextps()
                proj_fm(wc, "wc", c * 128, j, b)
                tt("dve", zc[:, c, tsl(j)], psum[b][:], zc[:, c, tsl(j)], ALU.mult, [("ps", b), ("zc", c)], [("ycv", c, j)])
        for j in range(NTT):
            gnorm(zc, "ycv", 2, C_GG, 6, sqs, rss, j)

        if "pool" not in PARTS:
            return
        P.barrier()
        R.reset()
        wa = load_w("wa", 0, 256)
        a = R.alloc("a", [128, 2, T], F32)
        t1 = R.alloc("t1", [128, T], F32)
        t2 = R.alloc("t2", [128, T], F32)
        dd = R.alloc("dd", [128, 2, T], BF16)
        pwbd = R.alloc("pwbd", [128, 2, 128], BF16)
        t16 = R.alloc("t16", [128, 16], F32)
        sqs = [R.alloc("asq", [128, 2, TT], BF16) for _ in range(2)]
        rss = [R.alloc("ars", [128, TT], F32) for _ in range(2)]
        memset("dve", pwbd[:], 0.0, ["pwbd"])
        for c in range(2):
            dma("pool", pwbd[0:64, c, 0:64], pw_d[l, 2 * c], "pw%da" % c, reads=["pwbd"], writes=[("pwbd", c, 0)])
            dma("pool", pwbd[64:128, c, 64:128], pw_d[l, 2 * c + 1], "pw%db" % c, reads=["pwbd"], writes=[("pwbd", c, 1)])
        for c in range(2):
            for j in range(NTT):
                b = nextps()
                proj_fm(wa, "wa", c * 128, j, b)
                act(a[:, c, tsl(j)], psum[b][:], AF.Copy, [("ps", b)], [("a", c, j)])
        for c in range(2):
            aall = [("a", c, j) for j in range(NTT)]
            ac = a[:, c, :]

            def shadd(dst, src, k, lo, hi, rd, wr, ac=ac):
                P.op("act", lambda e: e.copy(out=dst[lo:hi, 0:k], in_=src[lo:hi, 0:k]), rd + wr, wr)
                tt("dve", dst[lo:hi, k:T], src[lo:hi, k:T], src[lo:hi, 0:T - k], ALU.add, rd + wr, wr)

            shadd(t1, ac, 1, 0, 128, aall, ["t1"])
            if c == 0:
                shadd(t2, t1, 2, 64, 128, ["t1"], ["t2"])
                P.op("act", lambda e: e.copy(out=t2[0:64, :], in_=t1[0:64, :]), ["t1", "t2"], ["t2"])
            else:
                shadd(t2, t1, 2, 0, 128, ["t1"], ["t2"])
                shadd(t1, t2, 4, 0, 128, ["t2"], ["t1"])
                shadd(t2, t1, 8, 64, 128, ["t1"], ["t2"])
                P.op("act", lambda e: e.copy(out=t2[0:64, :], in_=t1[0:64, :]), ["t1", "t2"], ["t2"])
            stt("dve", dd[:, c, :], t2[:], par[:, C_INVW + c:C_INVW + c + 1], ac, ALU.mult, ALU.subtract,
                ["t2", "par"] + aall, [("dd", c)])
            tt("dve", t16[:], t2[:, 0:16], par[:, C_INVC + c * 16:C_INVC + (c + 1) * 16], ALU.mult, ["t2", "par", "t16"], ["t16"])
            tt("dve", dd[:, c, 0:16], t16[:], a[:, c, 0:16], ALU.subtract, ["t16", ("dd", c)] + aall, [("dd", c)])
        for j in range(NTT):
            for c in range(2):
                b = nextps()
                mm(psum[b][:], pwbd[:, c, :], dd[:, c, tsl(j)], True, True,
                   [("pwbd", c, 0), ("pwbd", c, 1), "pwbd", ("dd", c)], [("ps", b)])
                act(a[:, c, tsl(j)], psum[b][:], AF.Copy, [("ps", b), "par", ("dd", 0), ("dd", 1)], [("ya", c, j), ("a", c, j)],
                    scale=par[:, C_PSC + c:C_PSC + c + 1])
            gnorm(a, "ya", 2, C_GG, 0, sqs, rss, j)

        if "wout" not in PARTS:
            return
        wo = R.alloc("wo", [128, KC, D], BF16)
        dma("pool", wo[:], w_out_d[l].rearrange("(k p) n -> p k n", p=128), "wo", writes=["wo"])
        for j in range(NTT):
            for m in range(KC):
                b = nextps()
                for k in range(KC):
                    mm(psum[b][:], wo[:, k, m * 128:(m + 1) * 128], hcT[:, k, tsl(j)], k == 0, k == KC - 1,
                       ["wo", ("yc", k, j)], [("ps", b)])
                tt("dve", xT[:, m, tsl(j)], xT[:, m, tsl(j)], psum[b][:], ALU.add, [("ps", b), ("x", m, j)], [("x", m, j)])

    def ffn_phase(experts, use_hc):
        slots = []
        for s in range(2):
            wg = R.alloc("wg", [128, KC, FGW], BF16)
            wu = R.alloc("wu", [128, KC, FGW], BF16)
            wd = R.alloc("wd", [128, FGC, D], BF16)
            slots.append((wg, wu, wd))
        acts = [R.alloc("act", [128, FGC, TT], BF16) for _ in range(2)]
        sgs = [R.alloc("sg", [128, TT], F32) for _ in range(2)]
        gi = 0
        ai = 0
        si = 0
        for (wg_d, wu_d, wd_d, e, prep) in experts:
            if prep is not None:
                prep()
            for fg in range(NFG):
                s = gi % 2
                gi += 1
                wg, wu, wd = slots[s]
                c0 = fg * FGW
                dma("pool", wg[:], wg_d.rearrange("(k p) n -> p k n", p=128)[:, :, c0:c0 + FGW], "wg%d" % s, writes=[("wg", s)])
                dma("pool", wu[:], wu_d.rearrange("(k p) n -> p k n", p=128)[:, :, c0:c0 + FGW], "wu%d" % s, writes=[("wu", s)])
                dma("pool", wd[:], wd_d[c0:c0 + FGW, :].rearrange("(f p) n -> p f n", p=128), "wd%d" % s, writes=[("wd", s)])
                for j in range(NTT):
                    at = acts[ai % 2]
                    atag = ("act", ai % 2)
                    ai += 1
                    for f in range(FGC):
                        bg = nextps()
                        for k in range(KC):
                            mm(psum[bg][:], wg[:, k, f * 128:(f + 1) * 128], hT[:, k, tsl(j)], k == 0, k == KC - 1,
                               [("wg", s), ("h", k, j)], [("ps", bg)])
                        bu = nextps()
                        src = hcT if use_hc else hT
                        stag = "hc" if use_hc else "h"
                        for k in range(KC):
                            mm(psum[bu][:], wu[:, k, f * 128:(f + 1) * 128], src[:, k, tsl(j)], k == 0, k == KC - 1,
                               [("wu", s), (stag, k, j)], [("ps", bu)])
                        sg = sgs[si % 2]
                        sgt = ("sg", si % 2)
                        si += 1
                        act(sg[:], psum[bg][:], AF.Silu, [("ps", bg)], [sgt])
                        tt("dve", at[:, f, :], sg[:], psum[bu][:], ALU.mult, [sgt, ("ps", bu)], [(atag, f)])
                    for m in range(KC):
                        b = nextps()
                        for f in range(FGC):
                            mm(psum[b][:], wd[:, f, m * 128:(m + 1) * 128], at[:, f, :], f == 0, f == FGC - 1,
                               [("wd", s), (atag, f)], [("ps", b)])
                        tt("dve", xT[:, m, tsl(j)], xT[:, m, tsl(j)], psum[b][:], ALU.add, [("ps", b), ("x", m, j)], [("x", m, j)])

    def dense_ffn():
        R.reset()
        P.barrier()
        ffn_phase([(fwg_d, fwu_d, fwd_d, 0, None)], False)

    def moe_router_hook(kind, j=None, rs=None, tag=None, extra=None):
        if kind == "alloc":
            h2f = [R.alloc("h2f", [128, KC, TT], F32) for _ in range(1)]
            return h2f
        h2f = extra[0]
        pb = PL
        gcol = pb + 24
        for k in range(KC):
            stt("dve", h2f[:, k, :], xT[:, k, tsl(j)], par[:, gcol + k:gcol + k + 1], rs[:], ALU.mult, ALU.mult,
                [("x", k, j), "par", tag + "rs"], [("h2f", k)])
        for n in range(4):
            b = nextps()
            for k in range(KC):
                mm(psum[b][:, 0:NE], h2f[:, k, n * 128:(n + 1) * 128], rw[:, k, :], k == 0, k == KC - 1,
                   [("h2f", k), "rw"], [("ps", b)])
            tt("dve", comb[:, j * 4 + n, :], psum[b][:, 0:NE], par[:, C_RB:C_RB + NE], ALU.add, [("ps", b), "par"], [("lg", j * 4 + n)])
        for k in range(KC):
            P.op("act", lambda e, k=k: e.copy(out=hT[:, k, tsl(j)], in_=h2f[:, k, :]), [("h2f", k)], [("h", k, j)])
        if ROUTED:
            for n in range(4):
                for kq in range(2):
                    b = nextps()
                    for kk in range(4):
                        k = kq * 4 + kk
                        P.op("pe", lambda e, b=b, kk=kk, k=k, n=n: e.transpose(
                            out=psum[b][:, kk * 128:(kk + 1) * 128], in_=h2f[:, k, n * 128:(n + 1) * 128], identity=ident[:]),
                            [("h2f", k), "ident"], [("ps", b)])
                    P.op("act", lambda e, b=b, n=n, kq=kq: e.copy(out=hTok[:, j * 4 + n, kq * 512:(kq + 1) * 512], in_=psum[b][:]),
                         [("ps", b)], [("htok", j * 4 + n, kq)])

    CAP = 640
    NCH = CAP // 128
    CH = CAP // 2

    def moe_gating():
        P.barrier()
        R.reset()
        lgall = [("lg", i) for i in range(16)]
        m1 = R.alloc("m1", [128, 16], F32)
        m2 = R.alloc("m2", [128, 16], F32)
        eq1 = R.alloc("eq1", [128, 16, NE], F32)
        eq2 = R.alloc("eq2", [128, 16, NE], F32)
        lg2 = R.alloc("lg2", [128, 16, NE], F32)
        g1 = R.alloc("g1", [128, 16], F32)
        g2 = R.alloc("g2", [128, 16], F32)
        ind = R.alloc("ind", [128, 16, NE], F32)
        indb = R.alloc("indb", [128, 16, NE], BF16)
        pos = R.alloc("pos", [128, 16, NE], F32)
        tot = R.alloc("tot", [128, 16, NE], F32)
        boff = R.alloc("boff", [128, 16, NE], F32)
        cnt = R.alloc("cnt", [128, NE], F32)
        maxc = R.alloc("maxc", [128, 1], F32)
        hif = R.alloc("hif", [128, 16, NE], F32)
        ltf = R.alloc("ltf", [128, 128], F32)
        bc3 = lambda t_: t_[:].unsqueeze(2).to_broadcast([128, 16, NE])
        P.op("dve", lambda e: e.tensor_reduce(out=m1[:], in_=comb[:], axis=AX.X, op=ALU.max), lgall, ["m1"])
        tt("dve", eq1[:], comb[:], bc3(m1), ALU.is_equal, lgall + ["m1"], ["eq1"])
        stt("dve", lg2[:], eq1[:], -1e30, comb[:], ALU.mult, ALU.add, ["eq1"] + lgall, ["lg2"])
        P.op("dve", lambda e: e.tensor_reduce(out=m2[:], in_=lg2[:], axis=AX.X, op=ALU.max), ["lg2"], ["m2"])
        tt("dve", eq2[:], lg2[:], bc3(m2), ALU.is_equal, ["lg2", "m2"], ["eq2"])
        tt("dve", ind[:], eq1[:], eq2[:], ALU.add, ["eq1", "eq2"], ["ind"])
        tt("dve", g1[:], m1[:], m2[:], ALU.subtract, ["m1", "m2"], ["g1"])
        act(g1[:], g1[:], AF.Sigmoid, ["g1"], ["g1"])
        ts("dve", g2[:], g1[:], -1.0, 1.0, ALU.mult, ALU.add, ["g1"], ["g2"])
        tt("dve", eq1[:], eq1[:], bc3(g1), ALU.mult, ["eq1", "g1", "ind"], ["eq1"])
        tt("dve", eq2[:], eq2[:], bc3(g2), ALU.mult, ["eq2", "g2", "ind"], ["eq2"])
        tt("dve", comb[:], eq1[:], eq2[:], ALU.add, ["eq1", "eq2"] + lgall, ["comb"])
        if not ROUTED:
            memset("dve", flagf[:], 1.0, ["flagf"])
            P.op("dve", lambda e: e.tensor_copy(out=flagi[:], in_=flagf[:]), ["flagf"], ["flagi"])
            return
        memset("pool", ltf[:], 1.0, ["ltf"])
        P.op("pool", lambda e: e.affine_select(out=ltf[:], in_=ltf[:], compare_op=ALU.is_gt, fill=0.0,
                                               base=0, pattern=[[1, 128]], channel_multiplier=-1), ["ltf"], ["ltf"])
        P.op("act", lambda e: e.copy(out=ltri_bf[:], in_=ltf[:]), ["ltf"], ["ltri"])
        P.op("act", lambda e: e.copy(out=indb[:], in_=ind[:]), ["ind"], ["indb"])
        flat = lambda t_: t_[:].rearrange("p a b -> p (a b)")
        b = nextps()
        mm(psum[b][:, 0:128], ltri_bf[:], flat(indb), True, True, ["ltri", "indb"], [("ps", b)])
        P.op("dve", lambda e, b=b: e.tensor_copy(out=flat(pos), in_=psum[b][:, 0:128]), [("ps", b)], ["pos"])
        b = nextps()
        mm(psum[b][:, 0:128], ones_bf[:], flat(indb), True, True, ["ones_bf", "indb"], [("ps", b)])
        P.op("act", lambda e, b=b: e.copy(out=flat(tot), in_=psum[b][:, 0:128]), [("ps", b)], ["tot"])
        memset("dve", boff[:, 0, :], 0.0, ["boff"])
        for blk in range(1, 16):
            tt("dve", boff[:, blk, :], boff[:, blk - 1, :], tot[:, blk - 1, :], ALU.add, ["boff", "tot"], ["boff"])
        tt("dve", pos[:], pos[:], boff[:], ALU.add, ["pos", "boff"], ["pos"])
        tt("dve", cnt[:], boff[:, 15, :], tot[:, 15, :], ALU.add, ["boff", "tot"], ["cnt"])
        P.op("dve", lambda e: e.tensor_reduce(out=maxc[:], in_=cnt[:], axis=AX.X, op=ALU.max), ["cnt"], ["maxc"])
        ts("dve", flagf[:], maxc[:], float(CAP), 0.0, ALU.subtract, ALU.max, ["maxc"], ["flagf"])
        if FORCE_DENSE:
            ts("dve", flagf[:], flagf[:], 1.0, None, ALU.add, None, ["flagf"], ["flagf"])
        P.op("dve", lambda e: e.tensor_copy(out=flagi[:], in_=flagf[:]), ["flagf"], ["flagi"])
        ts("dve", pos[:], pos[:], 1.0, None, ALU.add, None, ["pos"], ["pos"])
        tt("dve", pos[:], pos[:], ind[:], ALU.mult, ["pos", "ind"], ["pos"])
        ts("dve", posm[:], pos[:], -1.0, None, ALU.add, None, ["pos"], ["posm"])
        P.op("dve", lambda e: e.tensor_copy(out=ghl[:, :, :, 0], in_=comb[:]), ["comb"], ["ghl0"])
        P.op("dve", lambda e: e.tensor_copy(out=hif[:], in_=ghl[:, :, :, 0]), ["ghl0"], ["hif"])
        tt("dve", ghl[:, :, :, 1], comb[:], hif[:], ALU.subtract, ["comb", "hif"], ["ghl1"])

    def moe_routed():
        Rr = Region(nc, hT_off, SB_END - hT_off)
        iota_row = Rr.alloc("iota_row", [128, CAP], F32)
        sid = Rr.alloc("sid", [128, 8], F32)
        sid0 = Rr.alloc("sid0", [128, 1], F32)
        diag = Rr.alloc("diag", [128, 128], F32)
        reps = [Rr.alloc("rep", [128, 128], F32) for _ in range(2)]
        g2s = Rr.alloc("g2s", [128, NCH, 2], F32)
        gsl = [Rr.alloc("gsl", [128, 8], F32) for _ in range(2)]
        slots = []
        for s_ in range(2):
            slots.append((Rr.alloc("rwg", [128, KC, 256], BF16), Rr.alloc("rwu", [128, KC, 256], BF16),
                          Rr.alloc("rwd", [128, 2, D], BF16)))
        acts = [Rr.alloc("ract", [128, 2, CAP], BF16) for _ in range(2)]
        sgs = [Rr.alloc("rsg", [128, CH], F32) for _ in range(2)]
        S = Rr.alloc("S", [128, 16, CAP], BF16)
        STs = [Rr.alloc("ST", [128, NCH, TT], BF16) for _ in range(2)]
        Xe = Rr.alloc("Xe", [128, KC, CAP], BF16)
        Ye = Rr.alloc("Ye", [128, NCH, D], F32)
        Yeb = Rr.alloc("Yeb", [128, NCH, D], BF16)
        b = nextps()
        mm(psum[b][:, 0:1], ltri_bf[:], ones_bf[:, 0:1], True, True, [], [("ps", b)])
        P.op("dve", lambda e, b=b: e.tensor_copy(out=sid0[:], in_=psum[b][:, 0:1]), [("ps", b)], ["sid0"])
        for c in range(NCH):
            ts("dve", sid[:, c:c + 1], sid0[:], float(128 * c), None, ALU.add, None, ["sid0"], [("sid", c)])
        ts("dve", diag[:], ident[:], sid0[:], None, ALU.mult, None, ["sid0"], ["diag"])
        b = nextps()
        mm(psum[b][:, 0:128], ones_f[:], diag[:], True, True, ["diag"], [("ps", b)])
        for c in range(NCH):
            ts("dve", iota_row[:, c * 128:(c + 1) * 128], psum[b][:, 0:128], float(128 * c), None, ALU.add, None,
               [("ps", b)], ["iota"])
        gi = si = sti = ri = 0
        NFG2 = DFF // 256
        for e in range(NE_RUN):
            wg_d = mwg_d[e].rearrange("(k p) n -> p k n", p=128)
            wu_d = mwu_d[e].rearrange("(k p) n -> p k n", p=128)
            for blk in range(16):
                ts("dve", S[:, blk, :], iota_row[:], posm[:, blk, e:e + 1], None, ALU.is_equal, None, ["iota"], [("S", blk)])
            for m in range(KC):
                for half in range(2):
                    b = nextps()
                    for blk in range(16):
                        mm(psum[b][:, 0:CH], hTok[:, blk, m * 128:(m + 1) * 128], S[:, blk, half * CH:(half + 1) * CH],
                           blk == 0, blk == 15, [("S", blk)], [("ps", b)])
                    P.op("act", lambda e_, b=b, m=m, half=half: e_.copy(out=Xe[:, m, half * CH:(half + 1) * CH], in_=psum[b][:, 0:CH]),
                         [("ps", b)], [("Xe", m, half)])
            gs = gsl[e % 2]
            gst = ("gsl", e % 2)
            b = nextps()
            for c in range(NCH):
                for blk in range(16):
                    mm(psum[b][:, 2 * c:2 * c + 2], S[:, blk, c * 128:(c + 1) * 128], ghl[:, blk, e, :], blk == 0, blk == 15,
                       [("S", blk)], [("ps", b)])
            P.op("act", lambda e_, b=b: e_.copy(out=g2s[:].rearrange("p c t -> p (c t)"), in_=psum[b][:, 0:2 * NCH]), [("ps", b)], ["g2s"])
            tt("dve", gs[:, 0:NCH], g2s[:, :, 0], g2s[:, :, 1], ALU.add, ["g2s"], [gst])
            for fg in range(NFG2):
                s_ = gi % 2
                at = acts[gi % 2]
                atag = ("ract", gi % 2)
                gi += 1
                wg, wu, wd = slots[s_]
                c0 = fg * 256
                dma("pool", wg[:], wg_d[:, :, c0:c0 + 256], "rwg%d" % s_, writes=[("rwg", s_)])
                dma("pool", wu[:], wu_d[:, :, c0:c0 + 256], "rwu%d" % s_, writes=[("rwu", s_)])
                dma("pool", wd[:], mwd_d[e][c0:c0 + 256, :].rearrange("(f p) n -> p f n", p=128), "rwd%d" % s_, writes=[("rwd", s_)])
                for f in range(2):
                    for half in range(2):
                        hs = slice(half * CH, (half + 1) * CH)
                        bg = nextps()
                        for k in range(KC):
                            mm(psum[bg][:, 0:CH], wg[:, k, f * 128:(f + 1) * 128], Xe[:, k, hs], k == 0, k == KC - 1,
                               [("rwg", s_), ("Xe", k, half)], [("ps", bg)])
                        bu = nextps()
                        for k in range(KC):
                            mm(psum[bu][:, 0:CH], wu[:, k, f * 128:(f + 1) * 128], Xe[:, k, hs], k == 0, k == KC - 1,
                               [("rwu", s_), ("Xe", k, half)], [("ps", bu)])
                        sg = sgs[si % 2]
                        sgt = ("rsg", si % 2)
                        si += 1
                        act(sg[:], psum[bg][:, 0:CH], AF.Silu, [("ps", bg)], [sgt])
                        tt("dve", at[:, f, hs], sg[:], psum[bu][:, 0:CH], ALU.mult, [sgt, ("ps", bu)], [(atag, f, half)])
                for c in range(NCH):
                    for fh in range(2):
                        b = nextps()
                        for f in range(2):
                            mm(psum[b][:], at[:, f, c * 128:(c + 1) * 128], wd[:, f, fh * 512:(fh + 1) * 512], f == 0, f == 1,
                               [("rwd", s_), (atag, f, 0), (atag, f, 1)], [("ps", b)])
                        ysl = Ye[:, c, fh * 512:(fh + 1) * 512]
                        ytag = ("Ye", c, fh)
                        gcol = gs[:, c:c + 1]
                        if fg == 0:
                            ts("dve", ysl, psum[b][:], gcol, None, ALU.mult, None, [("ps", b), gst], [ytag])
                        elif fg < NFG2 - 1:
                            stt("dve", ysl, psum[b][:], gcol, ysl, ALU.mult, ALU.add, [("ps", b), gst, ytag], [ytag])
                        else:
                            stt("dve", Yeb[:, c, fh * 512:(fh + 1) * 512], psum[b][:], gcol, ysl, ALU.mult, ALU.add,
                                [("ps", b), gst, ytag], [("Yeb", c, fh)])
            for n in range(NTT):
                STn = STs[sti % 2]
                stt_ = ("ST", sti % 2)
                sti += 1
                bp = nextps()
                for bl in range(4):
                    blk = n * 4 + bl
                    rp = reps[ri % 2]
                    rpt = ("rep", ri % 2)
                    ri += 1
                    P.op("dve", lambda e_, rp=rp, blk=blk, e=e: e_.tensor_copy(out=rp[:], in_=posm[:, blk, e:e + 1].to_broadcast([128, 128])),
                         [], [rpt])
                    mm(psum[bp][:, bl * 128:(bl + 1) * 128], rp[:], ident[:], True, True, [rpt], [("ps", bp)])
                for c in range(NCH):
                    ts("dve", STn[:, c, :], psum[bp][:], sid[:, c:c + 1], None, ALU.is_equal, None, [("ps", bp), ("sid", c)], [(stt_, c)])
                for m in range(KC):
                    b = nextps()
                    for c in range(NCH):
                        mm(psum[b][:], Yeb[:, c, m * 128:(m + 1) * 128], STn[:, c, :], c == 0, c == NCH - 1,
                           [("Yeb", c, m // 4), (stt_, c)], [("ps", b)])
                    tt("dve", xT[:, m, tsl(n)], xT[:, m, tsl(n)], psum[b][:], ALU.add, [("ps", b), ("x", m, n)], [("x", m, n)])

    def moe():
        R.reset()
        R.reset()
        ffn_bytes = 2 * (2 * KC * FGW * 2 + FGC * D * 2) + 2 * (FGC * TT * 2) + 2 * (TT * 4)
        tail = Region(nc, R.base + ffn_bytes, R.size - ffn_bytes - 2048)
        combT = tail.alloc("combT", [NE, T], F32)
        sel = tail.alloc("sel", [NE, NE, 128], F32)
        for e in range(NE):
            ts("dve", sel[:, e, :], ones_f[0:NE, :], ident[0:NE, e:e + 1], None, ALU.mult, None, ["ones_f", "ident"], [("sel", e)])
        for j in range(NTT):
            b = nextps()
            for n in range(4):
                P.op("pe", lambda e, b=b, n=n, j=j: e.transpose(out=psum[b][0:NE, n * 128:(n + 1) * 128],
                                                               in_=comb[:, j * 4 + n, :], identity=ident[:]),
                     ["comb", "ident"], [("ps", b)])
            P.op("act", lambda e, b=b, j=j: e.copy(out=combT[:, tsl(j)], in_=psum[b][0:NE, :]), [("ps", b)], [("combT", j)])

        def make_prep(e):
            def prep():
                for j in range(NTT):
                    b = nextps()
                    mm(psum[b][:], sel[:, e, :], combT[:, tsl(j)], True, True, [("sel", e), ("combT", j)], [("ps", b)])
                    for k in range(KC):
                        tt("dve", hcT[:, k, tsl(j)], hT[:, k, tsl(j)], psum[b][:], ALU.mult,
                           [("h", k, j), ("ps", b)], [("hc", k, j)])
            return prep

        experts = [(mwg_d[e], mwu_d[e], mwd_d[e], e, make_prep(e)) for e in range(NE_RUN)]
        ffn_phase(experts, True)

    out_idx = []

    def final_norm(raw=False):
        P.barrier()
        R.reset()
        sqs = [R.alloc("fsq", [128, KC, TT], BF16) for _ in range(2)]
        rss = [R.alloc("frs", [128, TT], F32) for _ in range(2)]
        obs = [R.alloc("fo", [128, KC, TT], F32) for _ in range(2)]
        outv = outT_d.rearrange("(k p) t -> p k t", p=128)
        for j in range(NTT):
            ob = obs[j % 2]
            otag = ("fo", j % 2)
            if raw:
                for k in range(KC):
                    P.op("act", lambda e, k=k, j=j, ob=ob: e.copy(out=ob[:, k, :], in_=xT[:, k, tsl(j)]), [("x", k, j)], [(otag, k)])
            else:
                sq, rs = sqs[j % 2], rss[j % 2]
                tag = "f%d" % (j % 2)
                group_rstd([(xT[:, k, tsl(j)], ("x", k, j)) for k in range(KC)], None, D, sq, rs, tag)
                for k in range(KC):
                    stt("dve", ob[:, k, :], xT[:, k, tsl(j)], par[:, C_FG + k:C_FG + k + 1], rs[:], ALU.mult, ALU.mult,
                        [("x", k, j), "par", tag + "rs"], [(otag, k)])
            out_idx.append(dma("sp", outv[:, :, tsl(j)], ob[:], "out%d" % (j % 2), reads=[(otag, k) for k in range(KC)]))

    rmsnorm_full(0 * PL + 0, hT, "h")
    mixer(0)
    if stage >= 2:
        rmsnorm_full(0 * PL + 24, hT, "h")
        dense_ffn()
    if stage >= 3:
        rmsnorm_full(1 * PL + 0, hT, "h")
        mixer(1)
    if stage >= 4:
        rmsnorm_full(1 * PL + 24, None, "h", hook=moe_router_hook)
        moe_gating()
        P.segment(1)
        if DEBUG:
            memset("dve", dbg[:, 1:2], 1.0, ["dbgm"])
        if ROUTED:
            moe_routed()
        P.segment(2)
        if DEBUG:
            memset("dve", dbg[:, 1:2], 2.0, ["dbgm"])
        moe()
        P.segment(3)
        if DEBUG:
            P.op("dve", lambda e: e.tensor_copy(out=dbg[:, 0:1], in_=flagf[:]), [], ["dbgf"])
            out_idx.append(dma("sp", dbg_d, dbg[:], "dbgo", reads=["dbgf"]))
    final_norm(raw=(stage < 99))
    nw = P.emit(final_waits=out_idx, flag_ap=flagi[0:1, 0:1], dummies=dummies)
    return nc, len(P.ops), nw


def host_layout(inputs):
    f = lambda a: np.ascontiguousarray(np.asarray(a, dtype=np.float32))
    g = {k: np.asarray(v) for k, v in inputs.items()}
    par = np.zeros((128, NPAR), np.float32)
    cols = lambda v, n: np.asarray(v, np.float32).reshape(n, 128).T
    for l in range(2):
        pb = l * PL
        par[:, pb:pb + 8] = cols(g["norm1_g"][l], 8)
        par[:, pb + 8:pb + 10] = cols(g["pool_scale"][l], 2)
        cw = np.asarray(g["conv_w"][l], np.float32)
        for c in range(2):
            for tap in range(3):
                par[:, pb + 10 + c * 3 + tap] = cw[tap, c * 128:(c + 1) * 128]
        par[:, pb + 16:pb + 24] = cols(g["group_g"][l], 8)
        par[:, pb + 24:pb + 32] = cols(g["norm2_g"][l], 8)
    par[:, C_FG:C_FG + 8] = cols(g["final_g"], 8)
    par[:, C_RB:C_RB + 8] = np.broadcast_to(np.asarray(g["router_b"][0], np.float32)[None, :], (128, 8))
    wins = np.array([[2, 4], [8, 16]], np.float32)
    for c in range(2):
        wcol = np.repeat(wins[c], 64)
        par[:, C_INVW + c] = 1.0 / wcol
        tpos = np.arange(16, dtype=np.float32)[None, :] + 1.0
        par[:, C_INVC + c * 16:C_INVC + (c + 1) * 16] = 1.0 / np.minimum(tpos, wcol[:, None])
    bc = np.zeros((2, 3, 128, 512), np.float32)
    for l in range(2):
        bc[l, 0] = np.broadcast_to(np.asarray(g["sgu_ln_g"][l], np.float32)[None, :], (128, 512))
        bc[l, 1] = np.broadcast_to(np.asarray(g["sgu_ln_b"][l], np.float32)[None, :], (128, 512))
        bc[l, 2] = np.broadcast_to(np.asarray(g["sgu_b"][l], np.float32).reshape(1, 512), (128, 512))
    shared = {
        "w_in": f(g["w_in"]), "w_out": f(g["w_out"]),
        "ffn_wg": f(g["ffn_w_gate"][0]), "ffn_wu": f(g["ffn_w_up"][0]), "ffn_wd": f(g["ffn_w_down"][0]),
        "moe_wg": f(g["moe_w_gate"][0]), "moe_wu": f(g["moe_w_up"][0]), "moe_wd": f(g["moe_w_down"][0]),
        "router_wl": f(np.asarray(g["router_w"][0], np.float32).reshape(KC, 128, NE).transpose(1, 0, 2)),
        "params": par, "bc": bc,
        "sgu_wT": f(np.asarray(g["sgu_w"], np.float32).transpose(0, 3, 1, 2)),
        "pool_w": f(g["pool_w"]),
    }
    x = np.asarray(g["x"], np.float32)
    in_maps = []
    for b in range(x.shape[0]):
        m = dict(shared)
        m["xT"] = np.ascontiguousarray(x[b].T)
        in_maps.append(m)
    return in_maps


_NC_CACHE = {}


def kernel(**inputs):
    in_maps = host_layout(inputs)
    if 99 not in _NC_CACHE:
        _NC_CACHE[99] = build_nc(99)[0]
    nc = _NC_CACHE[99]
    res = run_bass_kernel_spmd(nc, in_maps, core_ids=list(range(8)))
    out = np.stack([np.ascontiguousarray(r["outT"].T) for r in res.results], axis=0)
    return out.astype(np.float32)
```
